# Optimizing a Trainium2 kernel written in Bass

```python
import numpy as np
import jax, jax.numpy as jnp
from jax import lax

D_MODEL = 1024
BATCH = 8
SEQ = 4096
DEPTH = 1

HEAD_DIM = 64
N_FOX_HEADS = 8
N_NSA_HEADS = 8
N_NSA_KV = 2
NSA_GROUP = N_NSA_HEADS // N_NSA_KV
FOX_WIDTH = N_FOX_HEADS * HEAD_DIM
NSA_WIDTH = N_NSA_HEADS * HEAD_DIM
MIX_WIDTH = FOX_WIDTH + NSA_WIDTH
KV_WIDTH = N_NSA_KV * HEAD_DIM
CMP_LEN = 32
CMP_STRIDE = 16
CMP_HIDDEN = 256
SEL_LEN = 64
SEL_TOPK = 16
WINDOW = 512
Q_BLOCK = 128
SEL_Q_BLOCK = 64
D_FF = 4 * D_MODEL
ROPE_THETA = 10000.0
EPS = 1e-6
NEG = -1e30
FORCE_BONUS = 1e4
IN_SIZES = [FOX_WIDTH, FOX_WIDTH, FOX_WIDTH, N_FOX_HEADS,
            NSA_WIDTH,
            KV_WIDTH, KV_WIDTH,
            KV_WIDTH, KV_WIDTH,
            KV_WIDTH, KV_WIDTH,
            3 * N_NSA_HEADS]
N_IN = sum(IN_SIZES)

kernel_name = "hymba_fox_nsa_hybrid_layer"


def rmsnorm(x, g):
    xf = x.astype(jnp.float32)
    y = xf * lax.rsqrt(jnp.mean(xf * xf, axis=-1, keepdims=True) + EPS)
    return (y * g.astype(jnp.float32)).astype(x.dtype)


def rope(x, pos):
    half = HEAD_DIM // 2
    inv = ROPE_THETA ** (-jnp.arange(half, dtype=jnp.float32) / half)
    ang = pos.astype(jnp.float32)[:, None] * inv[None, :]
    cos = jnp.cos(ang)[:, None, :]
    sin = jnp.sin(ang)[:, None, :]
    xf = x.astype(jnp.float32)
    x1, x2 = xf[..., :half], xf[..., half:]
    out = jnp.concatenate([x1 * cos - x2 * sin, x1 * sin + x2 * cos], axis=-1)
    return out.astype(x.dtype)


def forgetting_attention(q, k, v, f_logit):
    B, S, H, dh = q.shape
    c = jnp.cumsum(jax.nn.log_sigmoid(f_logit.astype(jnp.float32)), axis=1)
    nq = S // Q_BLOCK
    qb = q.reshape(B, nq, Q_BLOCK, H, dh).transpose(1, 0, 2, 3, 4)
    cb = c.reshape(B, nq, Q_BLOCK, H).transpose(1, 0, 2, 3)
    starts = jnp.arange(nq) * Q_BLOCK
    kpos = jnp.arange(S)
    c_k = c.transpose(0, 2, 1)
    scale = HEAD_DIM ** -0.5

    def block(args):
        q_blk, c_blk, start = args
        qpos = start + jnp.arange(Q_BLOCK)
        s = jnp.einsum('bqhd,bkhd->bhqk', q_blk, k).astype(jnp.float32) * scale
        s = s + c_blk.transpose(0, 2, 1)[..., None] - c_k[:, :, None, :]
        s = jnp.where(kpos[None, :] <= qpos[:, None], s, NEG)
        p = jax.nn.softmax(s, axis=-1)
        return jnp.einsum('bhqk,bkhd->bqhd', p.astype(v.dtype), v)

    o = lax.map(block, (qb, cb, starts))
    return o.transpose(1, 0, 2, 3, 4).reshape(B, S, H, dh)


def compress(x_raw, pe, w1, b1, w2, b2):
    B, S, G, dh = x_raw.shape
    nc = (S - CMP_LEN) // CMP_STRIDE + 1
    idx = jnp.arange(nc)[:, None] * CMP_STRIDE + jnp.arange(CMP_LEN)[None, :]
    blk = x_raw[:, idx] + pe[None, None, :, None, :]
    blk = blk.transpose(0, 1, 3, 2, 4).reshape(B, nc, G, CMP_LEN * dh)
    h = jax.nn.gelu(blk @ w1 + b1)
    return h @ w2 + b2


def nsa_compressed_selected(q, k_cmp, v_cmp, k_slc, v_slc):
    B, S, H, dh = q.shape
    G = N_NSA_KV
    nc = k_cmp.shape[1]
    nb = S // SEL_LEN
    topk = min(SEL_TOPK, nb)
    scale = HEAD_DIM ** -0.5
    cmp_start = jnp.arange(nc) * CMP_STRIDE
    cmp_end = cmp_start + CMP_LEN - 1
    sel_start = jnp.arange(nb) * SEL_LEN
    overlap = ((cmp_start[:, None] < sel_start[None, :] + SEL_LEN)
               & (cmp_start[:, None] + CMP_LEN > sel_start[None, :])).astype(jnp.float32)
    k_t = k_slc.transpose(0, 2, 1, 3)
    v_t = v_slc.transpose(0, 2, 1, 3)
    b_ix = jnp.arange(B)[:, None, None]
    g_ix = jnp.arange(G)[None, :, None]
    jb = jnp.arange(nb)
    nq = S // SEL_Q_BLOCK
    qb = q.reshape(B, nq, SEL_Q_BLOCK, G, NSA_GROUP, dh).transpose(1, 0, 2, 3, 4, 5)
    starts = jnp.arange(nq) * SEL_Q_BLOCK

    def block(args):
        q_blk, start = args
        qpos = start + jnp.arange(SEL_Q_BLOCK)
        s = jnp.einsum('bqgnd,bcgd->bgnqc', q_blk, k_cmp).astype(jnp.float32) * scale
        vis = cmp_end[None, :] <= qpos[:, None]
        p_cmp = jnp.where(vis, jax.nn.softmax(jnp.where(vis, s, NEG), axis=-1), 0.0)
        o_cmp = jnp.einsum('bgnqc,bcgd->bqgnd', p_cmp.astype(v_cmp.dtype), v_cmp)
        imp = jnp.einsum('bgnqc,cj->bgqj', p_cmp, overlap)
        cur = qpos // SEL_LEN
        valid = jb[None, :] <= cur[:, None]
        forced = (jb[None, :] == 0) | (jb[None, :] == cur[:, None]) | (jb[None, :] == cur[:, None] - 1)
        score = jnp.where(valid, imp + jnp.where(forced, FORCE_BONUS, 0.0), -1.0)
        _, sel = lax.top_k(score, topk)
        kpos = (sel[..., None] * SEL_LEN + jnp.arange(SEL_LEN)).reshape(B, G, SEL_Q_BLOCK * topk * SEL_LEN)
        ks = k_t[b_ix, g_ix, kpos].reshape(B, G, SEL_Q_BLOCK, topk * SEL_LEN, dh)
        vs = v_t[b_ix, g_ix, kpos].reshape(B, G, SEL_Q_BLOCK, topk * SEL_LEN, dh)
        kpos = kpos.reshape(B, G, SEL_Q_BLOCK, topk * SEL_LEN)
        s2 = jnp.einsum('bqgnd,bgqkd->bgnqk', q_blk, ks).astype(jnp.float32) * scale
        mask = (kpos <= qpos[None, None, :, None])[:, :, None]
        p2 = jax.nn.softmax(jnp.where(mask, s2, NEG), axis=-1)
        o_slc = jnp.einsum('bgnqk,bgqkd->bqgnd', p2.astype(vs.dtype), vs)
        return o_cmp, o_slc

    o_cmp, o_slc = lax.map(block, (qb, starts))
    o_cmp = o_cmp.transpose(1, 0, 2, 3, 4, 5).reshape(B, S, H, dh)
    o_slc = o_slc.transpose(1, 0, 2, 3, 4, 5).reshape(B, S, H, dh)
    return o_cmp, o_slc


def nsa_window(q, k, v):
    B, S, H, dh = q.shape
    G = N_NSA_KV
    scale = HEAD_DIM ** -0.5
    span = WINDOW + Q_BLOCK
    kp = jnp.pad(k, ((0, 0), (WINDOW, 0), (0, 0), (0, 0)))
    vp = jnp.pad(v, ((0, 0), (WINDOW, 0), (0, 0), (0, 0)))
    nq = S // Q_BLOCK
    qb = q.reshape(B, nq, Q_BLOCK, G, NSA_GROUP, dh).transpose(1, 0, 2, 3, 4, 5)
    starts = jnp.arange(nq) * Q_BLOCK

    def block(args):
        q_blk, start = args
        kb = lax.dynamic_slice_in_dim(kp, start, span, axis=1)
        vb = lax.dynamic_slice_in_dim(vp, start, span, axis=1)
        qpos = start + jnp.arange(Q_BLOCK)
        kpos = start - WINDOW + jnp.arange(span)
        s = jnp.einsum('bqgnd,bkgd->bgnqk', q_blk, kb).astype(jnp.float32) * scale
        diff = qpos[:, None] - kpos[None, :]
        mask = (kpos[None, :] >= 0) & (diff >= 0) & (diff < WINDOW)
        p = jax.nn.softmax(jnp.where(mask, s, NEG), axis=-1)
        return jnp.einsum('bgnqk,bkgd->bqgnd', p.astype(vb.dtype), vb)

    o = lax.map(block, (qb, starts))
    return o.transpose(1, 0, 2, 3, 4, 5).reshape(B, S, H, dh)


def hybrid_mixer(h, w_in, b_f, b_gate, cmpk_pe, cmpk_w1, cmpk_b1, cmpk_w2, cmpk_b2,
                 cmpv_pe, cmpv_w1, cmpv_b1, cmpv_w2, cmpv_b2, g_fox, g_nsa, w_out):
    B, S, _ = h.shape
    pos = jnp.arange(S)
    proj = h @ w_in
    offsets = np.cumsum(IN_SIZES)[:-1].tolist()
    (q_f, k_f, v_f, f_logit, q_n, kc, vc, ks, vs, kw, vw, gate) = jnp.split(proj, offsets, axis=-1)
    fh = lambda t: t.reshape(B, S, N_FOX_HEADS, HEAD_DIM)
    o_fox = forgetting_attention(fh(q_f), fh(k_f), fh(v_f), f_logit + b_f)
    kvh = lambda t: t.reshape(B, S, N_NSA_KV, HEAD_DIM)
    q_n = rope(q_n.reshape(B, S, N_NSA_HEADS, HEAD_DIM), pos)
    k_cmp = compress(kvh(kc), cmpk_pe, cmpk_w1, cmpk_b1, cmpk_w2, cmpk_b2)
    nc = k_cmp.shape[1]
    k_cmp = rope(k_cmp, jnp.arange(nc) * CMP_STRIDE + CMP_LEN - 1)
    v_cmp = compress(kvh(vc), cmpv_pe, cmpv_w1, cmpv_b1, cmpv_w2, cmpv_b2)
    o_cmp, o_slc = nsa_compressed_selected(q_n, k_cmp, v_cmp, rope(kvh(ks), pos), kvh(vs))
    o_win = nsa_window(q_n, rope(kvh(kw), pos), kvh(vw))
    g = jax.nn.sigmoid((gate + b_gate).astype(jnp.float32)).reshape(B, S, N_NSA_HEADS, 3).astype(h.dtype)
    o_nsa = g[..., 0:1] * o_cmp + g[..., 1:2] * o_slc + g[..., 2:3] * o_win
    y = jnp.concatenate([rmsnorm(o_fox.reshape(B, S, FOX_WIDTH), g_fox),
                         rmsnorm(o_nsa.reshape(B, S, NSA_WIDTH), g_nsa)], axis=-1)
    return y @ w_out


def squared_relu_mlp(h, w_up, w_down):
    return jnp.square(jax.nn.relu(h @ w_up)) @ w_down


def setup_inputs(seed: int = 0) -> dict:
    key = jax.random.key(seed)
    ks = jax.random.split(key, 24)
    nrm = lambda k, shape, fan: jax.random.normal(k, shape, jnp.float32) * (fan ** -0.5)
    gain = lambda k, shape: 1.0 + 0.02 * jax.random.normal(k, shape, jnp.float32)
    small = lambda k, shape: 0.01 * jax.random.normal(k, shape, jnp.float32)
    L = DEPTH
    return {
        "x": jax.random.normal(ks[0], (BATCH, SEQ, D_MODEL), jnp.float32),
        "g_attn": gain(ks[1], (L, D_MODEL)),
        "w_in": nrm(ks[2], (L, D_MODEL, N_IN), D_MODEL),
        "b_f": jax.random.uniform(ks[3], (L, N_FOX_HEADS), jnp.float32, 1.0, 5.0),
        "b_gate": small(ks[4], (L, 3 * N_NSA_HEADS)),
        "cmpk_pe": 0.1 * jax.random.normal(ks[5], (L, CMP_LEN, HEAD_DIM), jnp.float32),
        "cmpk_w1": nrm(ks[6], (L, CMP_LEN * HEAD_DIM, CMP_HIDDEN), CMP_LEN * HEAD_DIM),
        "cmpk_b1": small(ks[7], (L, CMP_HIDDEN)),
        "cmpk_w2": nrm(ks[8], (L, CMP_HIDDEN, HEAD_DIM), CMP_HIDDEN),
        "cmpk_b2": small(ks[9], (L, HEAD_DIM)),
        "cmpv_pe": 0.1 * jax.random.normal(ks[10], (L, CMP_LEN, HEAD_DIM), jnp.float32),
        "cmpv_w1": nrm(ks[11], (L, CMP_LEN * HEAD_DIM, CMP_HIDDEN), CMP_LEN * HEAD_DIM),
        "cmpv_b1": small(ks[12], (L, CMP_HIDDEN)),
        "cmpv_w2": nrm(ks[13], (L, CMP_HIDDEN, HEAD_DIM), CMP_HIDDEN),
        "cmpv_b2": small(ks[14], (L, HEAD_DIM)),
        "g_fox": gain(ks[15], (L, FOX_WIDTH)),
        "g_nsa": gain(ks[16], (L, NSA_WIDTH)),
        "w_out": nrm(ks[17], (L, MIX_WIDTH, D_MODEL), MIX_WIDTH),
        "g_mlp": gain(ks[18], (L, D_MODEL)),
        "w_up": nrm(ks[19], (L, D_MODEL, D_FF), D_MODEL),
        "w_down": nrm(ks[20], (L, D_FF, D_MODEL), D_FF),
        "g_final": gain(ks[21], (D_MODEL,)),
    }


def reference(x, g_attn, w_in, b_f, b_gate, cmpk_pe, cmpk_w1, cmpk_b1, cmpk_w2, cmpk_b2,
              cmpv_pe, cmpv_w1, cmpv_b1, cmpv_w2, cmpv_b2, g_fox, g_nsa, w_out,
              g_mlp, w_up, w_down, g_final):
    h = x
    for l in range(DEPTH):
        h = h + hybrid_mixer(rmsnorm(h, g_attn[l]), w_in[l], b_f[l], b_gate[l],
                             cmpk_pe[l], cmpk_w1[l], cmpk_b1[l], cmpk_w2[l], cmpk_b2[l],
                             cmpv_pe[l], cmpv_w1[l], cmpv_b1[l], cmpv_w2[l], cmpv_b2[l],
                             g_fox[l], g_nsa[l], w_out[l])
        h = h + squared_relu_mlp(rmsnorm(h, g_mlp[l]), w_up[l], w_down[l])
    return rmsnorm(h, g_final)
```

```python
from contextlib import ExitStack
import numpy as np
import ml_dtypes
import concourse.bass as bass
import concourse.mybir as mybir
from concourse.bass_utils import run_bass_kernel_spmd

F32 = mybir.dt.float32
BF16 = mybir.dt.bfloat16
AF = mybir.ActivationFunctionType
ALU = mybir.AluOpType
AX = mybir.AxisListType
bf = ml_dtypes.bfloat16

S_LEN = 4096
D = 1024
NT = 32
EPS = 1e-6
NEGM = -30000.0
LIM = {}


class Sched:
    ENG = ('pe', 'act', 'dve', 'pool', 'sp')
    DMAQ = ('sp', 'act', 'pool')

    def __init__(self, nc, ndma=8, selfsync=True):
        self.nc = nc
        self.selfsync = selfsync
        self.semh = {}
        for e in self.ENG:
            self.semh['s_' + e] = nc.alloc_semaphore(name='s_' + e)
        self.ndma = {q: (ndma if q == 'sp' else 8) for q in self.DMAQ}
        for q in self.DMAQ:
            for i in range(self.ndma[q]):
                self.semh[f'd_{q}{i}'] = nc.alloc_semaphore(name=f'd_{q}{i}')
        self.cnt = {e: 0 for e in self.ENG}
        self.prog = {e: [] for e in self.ENG}
        self.seen = {e: {} for e in self.ENG}
        self.bufs = {}
        self.dma_use = {q: [0] * self.ndma[q] for q in self.DMAQ}
        self.dma_rr = {q: 0 for q in self.DMAQ}

    def _deps(self, eng, reads, writes):
        toks = []
        for b in reads:
            st = self.bufs.get(b)
            if st and st[0]:
                toks.append(st[0])
        for b in writes:
            st = self.bufs.get(b)
            if st:
                if st[0]:
                    toks.append(st[0])
                toks.extend(st[1])
        best = {}
        for (sk, val, teng) in toks:
            if teng == eng and (eng == 'pe' or not self.selfsync):
                continue
            if self.seen[eng].get(sk, 0) >= val:
                continue
            if best.get(sk, 0) < val:
                best[sk] = val
        for sk, val in best.items():
            self.seen[eng][sk] = val
        return list(best.items())

    def _mark(self, tok, reads, writes):
        for b in reads:
            self.bufs.setdefault(b, [None, []])[1].append(tok)
        for b in writes:
            self.bufs[b] = [tok, []]

    PSK = {'psT', 'pfm', 'prr', 'ptA', 'ptB', 'pcc', 'pS', 'pO', 'ptr', 'ph', 'pk2', 'pb1', 'pcz', 'pA', 'pU', 'pP'}

    def op(self, eng, fn, reads=(), writes=()):
        ex = [b for b in reads if (b if isinstance(b, str) else b[0]) in self.PSK]
        if ex:
            reads = [b for b in reads if b not in ex]
            writes = list(writes) + ex
        waits = self._deps(eng, reads, writes)
        self.cnt[eng] += 1
        tok = ('s_' + eng, self.cnt[eng], eng)
        self.prog[eng].append(('op', waits, fn))
        self._mark(tok, reads, writes)

    def dma(self, q, out, in_, reads=(), writes=()):
        i = self.dma_rr[q]
        self.dma_rr[q] = (i + 1) % self.ndma[q]
        sk = f'd_{q}{i}'
        prev = self.dma_use[q][i]
        waits = self._deps(q, reads, writes)
        if prev > 0 and self.seen[q].get(sk, 0) < 16 * prev:
            self.seen[q][sk] = 16 * prev
            waits = [w for w in waits if w[0] != sk] + [(sk, 16 * prev)]
        self.dma_use[q][i] = prev + 1
        tok = (sk, 16 * (prev + 1), 'dma')
        self.prog[q].append(('dma', waits, (out, in_, sk)))
        self._mark(tok, reads, writes)

    def _all_waits(self, eng, own=False):
        waits = []
        for e in self.ENG:
            if (e != eng or own) and self.cnt[e] > 0 and self.seen[eng].get('s_' + e, 0) < self.cnt[e]:
                self.seen[eng]['s_' + e] = self.cnt[e]
                waits.append(('s_' + e, self.cnt[e]))
        for q in self.DMAQ:
            for i in range(self.ndma[q]):
                v = 16 * self.dma_use[q][i]
                sk = f'd_{q}{i}'
                if v > 0 and self.seen[eng].get(sk, 0) < v:
                    self.seen[eng][sk] = v
                    waits.append((sk, v))
        return waits

    def barrier(self):
        cnt0 = dict(self.cnt)
        for e in self.ENG:
            w = self._all_waits(e, own=True)
            if w:
                self.prog[e].append(('wait', w, None))
        self.bufs = {}

    def finish(self):
        self.prog['sp'].append(('wait', self._all_waits('sp'), None))

    def emit(self):
        nc = self.nc
        with nc.Block() as block:
            def run(ename):
                def body(eng):
                    for kind, waits, payload in self.prog[ename]:
                        for sk, val in waits:
                            eng.wait_ge(self.semh[sk], val)
                        if kind == 'op':
                            ins = payload(eng)
                            ins.then_inc(self.semh['s_' + ename], 1)
                        elif kind == 'dma':
                            out, in_, sk = payload
                            eng.dma_start(out=out, in_=in_).then_inc(self.semh[sk], 16)
                return body
            block.tensor(run('pe'))
            block.scalar(run('act'))
            block.vector(run('dve'))
            block.gpsimd(run('pool'))
            block.sync(run('sp'))


def _consts():
    c = {}
    c['identb'] = np.eye(128, dtype=np.float32).astype(bf)
    c['identf'] = np.eye(128, dtype=np.float32)
    k = np.arange(128)[:, None]
    q = np.arange(128)[None, :]
    c['tri'] = (k <= q).astype(np.float32).astype(bf)
    c['atri'] = (k > q).astype(np.float32).astype(bf)
    c['U'] = (k <= q).astype(np.float32)
    c['onesf'] = np.ones((128, 128), np.float32)
    R = np.zeros((128, 128), np.float32)
    for m in range(128):
        if m % 64 < 32:
            R[m + 32, m] = -1.0
        else:
            R[m - 32, m] = 1.0
    c['R'] = R.astype(bf)
    half = 32
    inv = (10000.0 ** (-np.arange(half, dtype=np.float32) / half)).astype(np.float32)
    pos = np.arange(S_LEN, dtype=np.float32)
    ang = (pos[None, :] * inv[:, None]).astype(np.float32)
    c['cosT'] = np.tile(np.cos(ang).astype(np.float32), (4, 1))
    c['sinT'] = np.tile(np.sin(ang).astype(np.float32), (4, 1))
    posc = (np.arange(256, dtype=np.float32) * 16 + 31)
    angc = (posc[None, :] * inv[:, None]).astype(np.float32)
    c['cosC'] = np.tile(np.cos(angc).astype(np.float32), (4, 1))
    c['sinC'] = np.tile(np.sin(angc).astype(np.float32), (4, 1))
    s = np.arange(S_LEN)
    c['E'] = (s[None, :] // 64 == np.arange(64)[:, None]).astype(np.float32).astype(bf)
    cc = np.arange(256)
    vis = ((16 * cc[:, None] + 31 <= s[None, :]) & (cc[:, None] < 255)).astype(np.float32)
    c['vis'] = np.ascontiguousarray(vis.reshape(2, 128, S_LEN).transpose(1, 0, 2)).astype(bf)
    j = np.arange(64)
    ovl = ((16 * cc[:, None] < 64 * j[None, :] + 64) & (16 * cc[:, None] + 32 > 64 * j[None, :]) & (cc[:, None] < 255)).astype(np.float32)
    c['ovl'] = np.ascontiguousarray(ovl.reshape(2, 128, 64).transpose(1, 0, 2)).astype(bf)
    cur = s // 64
    valid = (j[None, :] <= cur[:, None])
    forced = (j[None, :] == 0) | (j[None, :] == cur[:, None]) | (j[None, :] == cur[:, None] - 1)
    bonus = np.where(valid, np.where(forced, 1e4, 0.0), -1.0).astype(np.float32)
    c['validm'] = np.ascontiguousarray(valid.astype(np.float32).reshape(32, 128, 64).transpose(1, 0, 2))
    c['bonus'] = np.ascontiguousarray(bonus.reshape(32, 128, 64).transpose(1, 0, 2))
    return c


IN_SPECS = [
    ('x', (S_LEN, D), F32), ('w_in', (D, 2848), F32), ('w_out', (D, D), F32), ('w_up', (D, 4096), F32),
    ('w_down', (4096, D), F32), ('g_attn', (1, D), F32), ('g_mlp', (1, D), F32), ('g_final', (1, D), F32),
    ('g_fox', (1, 512), F32), ('g_nsa', (1, 512), F32), ('b_f', (1, 8), F32), ('b_gate', (1, 24), F32),
    ('cmpk_peT', (64, 32), F32), ('cmpk_w1', (2048, 256), F32), ('cmpk_b1', (128, 2), F32), ('cmpk_w2', (256, 64), F32),
    ('cmpk_b2', (64, 1), F32),
    ('cmpv_peT', (64, 32), F32), ('cmpv_w1', (2048, 256), F32), ('cmpv_b1', (128, 2), F32), ('cmpv_w2', (256, 64), F32),
    ('cmpv_b2', (1, 64), F32),
    ('identb', (128, 128), BF16), ('identf', (128, 128), F32), ('tri', (128, 128), BF16), ('atri', (128, 128), BF16),
    ('U', (128, 128), F32), ('onesf', (128, 128), F32), ('R', (128, 128), BF16),
    ('cosT', (128, S_LEN), F32), ('sinT', (128, S_LEN), F32), ('cosC', (128, 256), F32), ('sinC', (128, 256), F32),
    ('E', (64, S_LEN), BF16), ('vis', (128, 2, S_LEN), BF16), ('ovl', (128, 2, 64), BF16),
    ('validm', (128, 32, 64), F32), ('bonus', (128, 32, 64), F32),
]


def build(phases=(1, 2, 3, 4), dbg=False):
    nc = bass.Bass("TRN2", target_bir_lowering=False)
    I = {}
    for name, shape, dt in IN_SPECS:
        I[name] = nc.dram_tensor(name, list(shape), dt, kind="ExternalInput").ap()
    out_d = nc.dram_tensor("out", [S_LEN, D], F32, kind="ExternalOutput").ap()
    skind = "ExternalOutput" if dbg else "Internal"
    FM = nc.dram_tensor("FM", [2048, S_LEN], BF16, kind=skind).ap()
    OS = nc.dram_tensor("OS", [S_LEN, D], F32, kind=skind).ap()
    DBG = nc.dram_tensor("DBG", [128, 2048], F32, kind=skind).ap()

    S = Sched(nc, ndma=LIM.get("ndma", 40))
    es_all = ExitStack()

    def sbt(es, name, shape, dt):
        return es.enter_context(nc.sbuf_tensor("sb_" + name, list(shape), dt)).ap()

    def pst(es, name, dt=F32):
        return es.enter_context(nc.psum_tensor("ps_" + name, [128, 2048 // (4 if dt == F32 else 2)], dt)).ap()

    def mm(out, lhsT, rhs, start=True, stop=True, r=(), w=()):
        S.op('pe', lambda e: e.matmul(out, lhsT=lhsT, rhs=rhs, start=start, stop=stop), reads=r, writes=w)

    def tp(out, in_, ident, r=(), w=()):
        S.op('pe', lambda e: e.transpose(out=out, in_=in_, identity=ident), reads=r, writes=w)

    def act(out, in_, func, bias=0.0, scale=1.0, r=(), w=()):
        S.op('act', lambda e: e.activation(out=out, in_=in_, func=func, bias=bias, scale=scale), reads=r, writes=w)

    def cp(eng, out, in_, r=(), w=()):
        if eng == 'act':
            S.op('act', lambda e: e.copy(out=out, in_=in_), reads=r, writes=w)
        else:
            S.op(eng, lambda e: e.tensor_copy(out=out, in_=in_), reads=r, writes=w)

    def tt(eng, out, in0, in1, op, r=(), w=()):
        S.op(eng, lambda e: e.tensor_tensor(out=out, in0=in0, in1=in1, op=op), reads=r, writes=w)

    def ts(eng, out, in0, s1, op0, s2=None, op1=None, r=(), w=()):
        if op1 is None:
            S.op(eng, lambda e: e.tensor_scalar(out=out, in0=in0, scalar1=s1, scalar2=None, op0=op0), reads=r, writes=w)
        else:
            S.op(eng, lambda e: e.tensor_scalar(out=out, in0=in0, scalar1=s1, scalar2=s2, op0=op0, op1=op1), reads=r, writes=w)

    def stt(eng, out, in0, sc, in1, op0, op1, r=(), w=()):
        S.op(eng, lambda e: e.scalar_tensor_tensor(out=out, in0=in0, scalar=sc, in1=in1, op0=op0, op1=op1), reads=r, writes=w)

    def red(eng, out, in_, op, r=(), w=()):
        S.op(eng, lambda e: e.tensor_reduce(out=out, in_=in_, axis=AX.X, op=op), reads=r, writes=w)

    def recip(out, in_, r=(), w=()):
        S.op('dve', lambda e: e.reciprocal(out=out, in_=in_), reads=r, writes=w)

    def mset(eng, ap, val, w=()):
        S.op(eng, lambda e: e.memset(ap, val), writes=w)

    def dma(out, in_, r=(), w=(), q='sp'):
        S.dma(q, out, in_, reads=r, writes=w)

    def rmsstat(x_ap, n, sq, ss, rstd, rk, tag):
        tt('dve', sq, x_ap, x_ap, ALU.mult, r=rk, w=[tag + 'sq'])
        red('dve', ss, sq, ALU.add, r=[tag + 'sq'], w=[tag + 'ss'])
        act(rstd, ss, AF.Ln, bias=epsb[:, 0:1], scale=1.0 / n, r=[tag + 'ss', 'epsb'], w=[tag + 'rstd'])
        act(rstd, rstd, AF.Exp, scale=-0.5, r=[tag + 'rstd'], w=[tag + 'rstd'])

    P = es_all
    identb = sbt(P, "identb", [128, 128], BF16)
    identf = sbt(P, "identf", [128, 128], F32)
    tri = sbt(P, "tri", [128, 128], BF16)
    atri = sbt(P, "atri", [128, 128], BF16)
    epsb = sbt(P, "epsb", [128, 2], F32)
    for nm, t in (('identb', identb), ('identf', identf), ('tri', tri), ('atri', atri)):
        dma(t, I[nm], w=[nm])
    mset('dve', epsb[:, 0:1], EPS, w=['epsb'])
    mset('dve', epsb[:, 1:2], 1.0, w=['epsb'])

    if 1 in phases or 2 in phases or 3 in phases:
        es12 = ExitStack()
        VF = sbt(es12, "VF", [128, NT, 8, 65], BF16)
        logf = sbt(es12, "logf", [128, NT, 8], F32)
        gates = sbt(es12, "gates", [128, NT, 24], F32)
        negc = sbt(es12, "negc", [128, NT, 8], F32)
        chi = sbt(es12, "chi", [8, S_LEN], BF16)
        clo = sbt(es12, "clo", [8, S_LEN], BF16)
        esV = ExitStack()
        VS = sbt(esV, "VS", [128, NT, 2, 65], BF16)
        VW = sbt(esV, "VW", [128, NT, 2, 65], BF16)
        mset('pool', logf.rearrange("p a b -> p (a b)"), 0.0, w=['logf'])
        mset('pool', gates.rearrange("p a b -> p (a b)"), 0.0, w=['gates'])
        mset('pool', VF.rearrange("p a b c -> p (a b c)"), 1.0, w=['VF'])
        mset('pool', VS.rearrange("p a b c -> p (a b c)"), 1.0, w=['VS'])
        mset('pool', VW.rearrange("p a b c -> p (a b c)"), 1.0, w=['VW'])

    if 1 in phases:
        with ExitStack() as es:
            Wfm = sbt(es, "Wfm", [128, 8, 2048], BF16)
            Wtm = sbt(es, "Wtm", [128, 8, 800], BF16)
            wst = [sbt(es, f"wst{i}", [128, 2848], F32) for i in range(2)]
            gat = sbt(es, "gat", [128, D], F32)
            bfb = sbt(es, "bfb", [128, 8], F32)
            bgb = sbt(es, "bgb", [128, 24], F32)
            Rm = sbt(es, "Rm", [128, 128], BF16)
            Um = sbt(es, "Um", [128, 128], F32)
            On = sbt(es, "On", [128, 128], F32)
            xt = [sbt(es, f"xt{i}", [128, D], F32) for i in range(2)]
            sq = sbt(es, "sq", [128, D], F32)
            ss = sbt(es, "ss", [128, 1], F32)
            rstd = sbt(es, "rstd", [128, 1], F32)
            hnb = [sbt(es, f"hnb{i}", [128, D], BF16) for i in range(2)]
            hnT = [sbt(es, f"hnT{i}", [128, 8, 512], BF16) for i in range(2)]
            fmo = [sbt(es, f"fmo{i}", [128, 512], BF16) for i in range(4)]
            cs = [sbt(es, f"cs{i}", [128, 2, 512], F32) for i in range(2)]
            xb = [sbt(es, f"xb{i}", [128, 512], BF16) for i in range(2)]
            t1 = [sbt(es, f"t1{i}", [128, 512], F32) for i in range(2)]
            t2 = [sbt(es, f"t2{i}", [128, 512], F32) for i in range(2)]
            zt = sbt(es, "zt", [128, 32], F32)
            Xp = sbt(es, "Xp", [128, NT, 8], F32)
            psT = pst(es, "psT", BF16)
            pfm = [pst(es, f"pfm{i}") for i in range(2)]
            prr = pst(es, "prr")
            ptA = [pst(es, f"ptA{i}") for i in range(2)]
            ptB = [pst(es, f"ptB{i}") for i in range(2)]
            pcc = ptA[0]

            dma(gat, I['g_attn'].partition_broadcast(128), w=['gat'])
            dma(bfb, I['b_f'].partition_broadcast(128), w=['bfb'])
            dma(bgb, I['b_gate'].partition_broadcast(128), w=['bgb'])
            dma(Rm, I['R'], w=['Rm'])
            dma(Um, I['U'], w=['Um'])
            dma(On, I['onesf'], w=['On'])
            fm_src = [(0, 1024, 0), (1544, 2056, 1024), (2056, 2312, 1536), (2312, 2440, 1792), (2568, 2696, 1920)]
            tm_src = [(1024, 1536, 0), (2440, 2568, 512), (2696, 2824, 640), (1536, 1544, 768), (2824, 2848, 776)]
            pieces = [('Wfm', 0, 512, 0, 'pool'), ('Wfm', 512, 1024, 512, 'dve'), ('Wfm', 1544, 2056, 1024, 'act'),
                      ('Wfm', 2056, 2312, 1536, 'dve'), ('Wfm', 2312, 2440, 1792, 'act'), ('Wfm', 2568, 2696, 1920, 'act'),
                      ('Wtm', 1024, 1536, 0, 'pool'), ('Wtm', 2440, 2568, 512, 'act'), ('Wtm', 2696, 2824, 640, 'dve'),
                      ('Wtm', 1536, 1544, 768, 'dve'), ('Wtm', 2824, 2848, 776, 'dve')]
            for kc in range(8):
                st = wst[kc % 2]
                dma(st, I['w_in'][kc * 128:(kc + 1) * 128, :], w=[('wst', kc % 2)])
                for pi, (wn, a_, b_, d0, eng) in enumerate(pieces):
                    dst = (Wfm if wn == 'Wfm' else Wtm)[:, kc, d0:d0 + (b_ - a_)]
                    S.op(eng, (lambda e, dst=dst, src=st[:, a_:b_]: e.copy(out=dst, in_=src)) if eng == 'act'
                         else (lambda e, dst=dst, src=st[:, a_:b_]: e.tensor_copy(out=dst, in_=src)),
                         reads=[('wst', kc % 2)], writes=[(wn, kc, pi)])
            ROPE = {8, 9, 10, 11, 14, 15}
            NCH = LIM.get('p1', 8)
            cnt = {'fi': 0, 'ri': 0}

            def cs_load(ci):
                c_s = ci % 2
                dma(cs[c_s][:, 0, :], I['cosT'][:, ci * 512:(ci + 1) * 512], w=[('cs', c_s, 0)])
                dma(cs[c_s][:, 1, :], I['sinT'][:, ci * 512:(ci + 1) * 512], w=[('cs', c_s, 1)])

            def prologue_tile(ci, t4):
                c_s = ci % 2
                kt = ci * 4 + t4
                s2 = kt % 2
                dma(xt[s2], I['x'][kt * 128:(kt + 1) * 128, :], w=[('xt', s2)])
                rmsstat(xt[s2], D, sq, ss, rstd, [('xt', s2)], 'p1')
                stt('dve', hnb[s2], xt[s2], rstd[:, 0:1], gat, ALU.mult, ALU.mult, r=[('xt', s2), 'p1rstd', 'gat'], w=[('hnb', s2)])
                for kc in range(8):
                    tp(psT[:, kc * 128:(kc + 1) * 128], hnb[s2][:, kc * 128:(kc + 1) * 128], identb, r=[('hnb', s2), 'identb'], w=['psT'])
                cp('act', hnT[c_s][:, :, t4 * 128:(t4 + 1) * 128], psT.rearrange("p (k t) -> p k t", k=8), r=['psT'], w=[('hnT', c_s)])

            if NCH > 0:
                cs_load(0)
                for t4 in range(4):
                    prologue_tile(0, t4)
            S.barrier()
            for ci in range(NCH):
                c_s = ci % 2
                if ci + 1 < NCH:
                    cs_load(ci + 1)
                pend_rot = []

                def rot_part2(pb_unused, r2, fo, fk, mt, ci=ci, c_s=c_s):
                    mm(prr, Rm, xb[r2], r=['Rm', ('xb', r2)], w=['prr'])
                    tt('dve', t2[r2], prr, cs[c_s][:, 1, :], ALU.mult, r=['prr', ('cs', c_s, 1)], w=[('t2', r2)])
                    tt('pool', fo, t1[r2], t2[r2], ALU.add, r=[('t1', r2), ('t2', r2)], w=[fk])
                    dma(FM[mt * 128:(mt + 1) * 128, ci * 512:(ci + 1) * 512], fo, r=[fk], w=[('FM', mt, ci)], q='pool')
                for mt in range(16):
                    pb = pfm[mt % 2]
                    pk = ('pfm', mt % 2)
                    for kc in range(8):
                        mm(pb, Wfm[:, kc, mt * 128:(mt + 1) * 128], hnT[c_s][:, kc, :], start=(kc == 0), stop=(kc == 7),
                           r=['Wfm', ('hnT', c_s)], w=[pk])
                    while pend_rot:
                        rot_part2(*pend_rot.pop(0))
                    fo = fmo[cnt['fi'] % 4]
                    fk = ('fmo', cnt['fi'] % 4)
                    cnt['fi'] += 1
                    if mt not in ROPE:
                        cp('act', fo, pb, r=[pk], w=[fk])
                        dma(FM[mt * 128:(mt + 1) * 128, ci * 512:(ci + 1) * 512], fo, r=[fk], w=[('FM', mt, ci)], q='act')
                    else:
                        r2 = cnt['ri'] % 2
                        cnt['ri'] += 1
                        cp('act', xb[r2], pb, r=[pk], w=[('xb', r2)])
                        tt('dve', t1[r2], pb, cs[c_s][:, 0, :], ALU.mult, r=[pk, ('cs', c_s, 0)], w=[('t1', r2)])
                        pend_rot.append((None, r2, fo, fk, mt))
                    if ci + 1 < NCH and mt in (3, 7, 11, 15):
                        prologue_tile(ci + 1, mt // 4)
                while pend_rot:
                    rot_part2(*pend_rot.pop(0))
                for t4 in range(4):
                    kt = ci * 4 + t4
                    pA_ = ptA[t4 % 2]
                    pB_ = ptB[t4 % 2]
                    ka = ('ptA', t4 % 2)
                    kb = ('ptB', t4 % 2)
                    for kc in range(8):
                        mm(pA_, hnT[c_s][:, kc, t4 * 128:(t4 + 1) * 128], Wtm[:, kc, 0:512], start=(kc == 0), stop=(kc == 7),
                           r=['Wtm', ('hnT', c_s)], w=[ka])
                    for kc in range(8):
                        mm(pB_[:, 0:288], hnT[c_s][:, kc, t4 * 128:(t4 + 1) * 128], Wtm[:, kc, 512:800], start=(kc == 0), stop=(kc == 7),
                           r=['Wtm', ('hnT', c_s)], w=[kb])
                    cp('act', VF[:, kt, :, 0:64], pA_.rearrange("p (h d) -> p h d", h=8), r=[ka], w=['VF'])
                    cp('act', VS[:, kt, :, 0:64], pB_[:, 0:128].rearrange("p (h d) -> p h d", h=2), r=[kb], w=['VS'])
                    cp('act', VW[:, kt, :, 0:64], pB_[:, 128:256].rearrange("p (h d) -> p h d", h=2), r=[kb], w=['VW'])
                    tt('dve', zt[:, 0:8], pB_[:, 256:264], bfb, ALU.add, r=[kb, 'bfb'], w=['zt'])
                    tt('dve', zt[:, 8:32], pB_[:, 264:288], bgb, ALU.add, r=[kb, 'bgb'], w=['zt'])
                    act(zt, zt, AF.Exp, scale=-1.0, r=['zt'], w=['zt'])
                    act(logf[:, kt, :], zt[:, 0:8], AF.Ln, bias=epsb[:, 1:2], r=['zt', 'epsb'], w=['logf'])
                    ts('dve', logf[:, kt, :], logf[:, kt, :], -1.0, ALU.mult, r=['logf'], w=['logf'])
                    ts('dve', zt[:, 8:32], zt[:, 8:32], 1.0, ALU.add, r=['zt'], w=['zt'])
                    recip(gates[:, kt, :], zt[:, 8:32], r=['zt'], w=['gates'])
            mset('dve', Xp[:, 0, :], 0.0, w=['Xp'])
            for kt in range(1, NT if LIM.get('stage', 9) >= 3 else 1):
                tt('dve', Xp[:, kt, :], Xp[:, kt - 1, :], logf[:, kt - 1, :], ALU.add, r=['Xp', 'logf'], w=['Xp'])
            if LIM.get('stage', 9) < 4:
                S.finish(); S.emit(); return nc
            lf2 = logf.rearrange("p k h -> p (k h)")
            xp2 = Xp.rearrange("p k h -> p (k h)")
            mm(pcc[:, 0:256], Um, lf2, start=True, stop=False, r=['Um', 'logf'], w=[('ptA', 0)])
            mm(pcc[:, 0:256], On, xp2, start=False, stop=True, r=['On', 'Xp'], w=[('ptA', 0)])
            ts('dve', negc.rearrange("p k h -> p (k h)"), pcc[:, 0:256], -1.0, ALU.mult, r=[('ptA', 0)], w=['negc'])
            for kg in range(8):
                pb = pfm[kg % 2]
                pk = ('pfm', kg % 2)
                for j4 in range(4):
                    kt = kg * 4 + j4
                    mm(pb[0:8, j4 * 128:(j4 + 1) * 128], logf[:, kt, :], Um, start=True, stop=False, r=['Um', 'logf'], w=[pk])
                    mm(pb[0:8, j4 * 128:(j4 + 1) * 128], Xp[:, kt, :], On, start=False, stop=True, r=['On', 'Xp'], w=[pk])
                ts('dve', chi[:, kg * 512:(kg + 1) * 512], pb[0:8, :], 8.0, ALU.mult, r=[pk], w=['chi'])
                stt('dve', clo[:, kg * 512:(kg + 1) * 512], pb[0:8, :], 8.0, chi[:, kg * 512:(kg + 1) * 512], ALU.mult, ALU.subtract,
                    r=[pk, 'chi'], w=['clo'])
            if dbg:
                dma(DBG[:, 0:256], negc.rearrange("p k h -> p (k h)"), r=['negc'])
                dma(DBG[:, 256:1024], gates.rearrange("p k h -> p (k h)"), r=['gates'])
        S.barrier()

    def finalize(po, osb, osk, ptr, ptrk, rz, otok, otk, gate_ap=None):
        cp('dve', osb[0:65, :], po[0:65, :], r=[osk[0]], w=[osk[1]])
        for j in range(4):
            tp(ptr[:, j * 65:(j + 1) * 65], osb[0:65, j * 128:(j + 1) * 128], identf[0:65, 0:65], r=[osk[1], 'identf'], w=[ptrk])
        pv = ptr[:, 0:260].rearrange("p (j d) -> p j d", j=4)
        recip(rz[:, 0:4], pv[:, :, 64], r=[ptrk], w=['rz'])
        if gate_ap is not None:
            tt('dve', rz[:, 0:4], rz[:, 0:4], gate_ap, ALU.mult, r=['rz', 'gates'], w=['rz'])
        tt('dve', otok, pv[:, :, 0:64], rz[:, 0:4].unsqueeze(2).broadcast_to([128, 4, 64]), ALU.mult, r=[ptrk, 'rz'], w=[otk])

    if 3 in phases:
        with ExitStack() as es:
            KSA = [sbt(es, f"KSA{g}", [128, S_LEN], BF16) for g in range(2)]
            KW = [sbt(es, f"KW{g}", [64, S_LEN], BF16) for g in range(2)]
            KCT = [sbt(es, f"KCT{g}", [64, 256], BF16) for g in range(2)]
            VOZ = [sbt(es, f"VOZ{g}", [128, 2, 129], BF16) for g in range(2)]
            vis = sbt(es, "vis", [128, 2, S_LEN], BF16)
            validm = sbt(es, "validm", [128, 32, 64], F32)
            bonus = sbt(es, "bonus", [128, 32, 64], F32)
            Rm = sbt(es, "Rm3", [128, 128], BF16)
            for g in range(2):
                dma(KSA[g][0:64, :], FM[1792 + g * 64:1792 + (g + 1) * 64, :], r=['FM'], w=[('KSA', g)])
                dma(KSA[g][64:128, :], I['E'], w=[('KSAe', g)])
                dma(KW[g], FM[1920 + g * 64:1920 + (g + 1) * 64, :], r=['FM'], w=[('KW', g)])
                mset('pool', VOZ[g], 0.0, w=[('VOZ', g)])
            dma(Rm, I['R'], w=['Rm'])
            with ExitStack() as ec:
                XC = sbt(ec, "XC", [128, S_LEN], BF16)
                W1 = sbt(ec, "W1", [128, 32, 256], BF16)
                W1s2 = [sbt(ec, f"W1s{i}", [128, 8, 256], F32) for i in range(2)]
                W2s = sbt(ec, "W2s", [128, 2, 64], F32)
                W2 = sbt(ec, "W2", [128, 2, 64], BF16)
                peT = sbt(ec, "peT", [128, 32], F32)
                peTb = sbt(ec, "peTb", [128, 32], BF16)
                b1 = sbt(ec, "b1", [128, 2], F32)
                btot = sbt(ec, "btot", [128, 2], F32)
                b2c = sbt(ec, "b2c", [64, 1], F32)
                b2r = sbt(ec, "b2r", [128, 64], F32)
                hT = [sbt(ec, f"hT{i}", [128, 256], BF16) for i in range(4)]
                xg = sbt(ec, "xg", [128, 256], F32)
                ug = sbt(ec, "ug", [128, 256], F32)
                kx = sbt(ec, "kx", [64, 256], BF16)
                cC = sbt(ec, "cC", [64, 2, 256], F32)
                k1 = sbt(ec, "k1", [64, 256], F32)
                k2 = sbt(ec, "k2", [64, 256], F32)
                ph = [pst(ec, f"ph{i}") for i in range(2)]
                pk2 = pst(ec, "pk2")
                pb1 = pst(ec, "pb1")
                dma(cC[:, 0, :], I['cosC'][0:64, :], w=['cC0'])
                dma(cC[:, 1, :], I['sinC'][0:64, :], w=['cC1'])
                for which in ('k', 'v'):
                    pre = 'cmp' + which
                    dma(XC, FM[1536:1664, :] if which == 'k' else FM[1664:1792, :], r=['FM'], w=['XC'])
                    w1v = I[pre + '_w1'].rearrange("(l d) h -> d l h", d=64)
                    for lq in range(4):
                        W1s = W1s2[lq % 2]
                        for half in range(2):
                            dma(W1s[half * 64:(half + 1) * 64, :, :], w1v[:, lq * 8:(lq + 1) * 8, :], w=[('W1s', lq % 2, half)])
                        cp('pool' if lq % 2 == 0 else 'dve', W1[:, lq * 8:(lq + 1) * 8, :], W1s, r=[('W1s', lq % 2, 0), ('W1s', lq % 2, 1)], w=[('W1', lq)])
                    dma(W2s, I[pre + '_w2'].rearrange("(c p) d -> p c d", p=128), w=['W2s'])
                    cp('pool', W2, W2s, r=['W2s'], w=['W2'])
                    for half in range(2):
                        dma(peT[half * 64:(half + 1) * 64, :], I[pre + '_peT'], w=[('peT', half)])
                    cp('pool', peTb, peT, r=[('peT', 0), ('peT', 1)], w=['peTb'])
                    dma(b1, I[pre + '_b1'], w=['b1'])
                    if which == 'k':
                        dma(b2c, I['cmpk_b2'], w=['b2c'])
                    else:
                        dma(b2r, I['cmpv_b2'].partition_broadcast(128), w=['b2r'])
                    for hc in range(2):
                        for l in range(32):
                            mm(pb1[:, hc:hc + 1], W1[0:64, l, hc * 128:(hc + 1) * 128], peTb[0:64, l:l + 1], start=(l == 0), stop=(l == 31),
                               r=[('W1', 0), ('W1', 1), ('W1', 2), ('W1', 3), 'peTb'], w=['pb1'])
                    tt('dve', btot, pb1[:, 0:2], b1, ALU.add, r=['pb1', 'b1'], w=['btot'])
                    xcv = XC.rearrange("p (c s) -> p c s", s=16)
                    for g in range(2):
                        for hc in range(2):
                            pp = ph[hc]
                            for l in range(32):
                                rhs = xcv[g * 64:(g + 1) * 64, 0:255, l] if l < 16 else xcv[g * 64:(g + 1) * 64, 1:256, l - 16]
                                mm(pp[:, 0:255], W1[g * 64:(g + 1) * 64, l, hc * 128:(hc + 1) * 128], rhs, start=(l == 0), stop=(l == 31),
                                   r=[('W1', 0), ('W1', 1), ('W1', 2), ('W1', 3), 'XC'], w=[('ph', hc)])
                            hh = hT[g * 2 + hc]
                            hk = ('hT', g * 2 + hc)
                            ts('dve', xg[:, 0:255], pp[:, 0:255], btot[:, hc:hc + 1], ALU.add, r=[('ph', hc), 'btot'], w=['xg'])
                            tt('dve', ug[:, 0:255], xg[:, 0:255], xg[:, 0:255], ALU.mult, r=['xg'], w=['ug'])
                            ts('dve', ug[:, 0:255], ug[:, 0:255], 0.044715, ALU.mult, 1.0, ALU.add, r=['ug'], w=['ug'])
                            tt('dve', ug[:, 0:255], ug[:, 0:255], xg[:, 0:255], ALU.mult, r=['ug', 'xg'], w=['ug'])
                            act(ug[:, 0:255], ug[:, 0:255], AF.Exp, scale=-1.59576912, r=['ug'], w=['ug'])
                            ts('dve', ug[:, 0:255], ug[:, 0:255], 1.0, ALU.add, r=['ug'], w=['ug'])
                            recip(ug[:, 0:255], ug[:, 0:255], r=['ug'], w=['ug'])
                            tt('dve', hh[:, 0:255], xg[:, 0:255], ug[:, 0:255], ALU.mult, r=['ug', 'xg'], w=[hk])
                        if which == 'k':
                            for hc in range(2):
                                mm(pk2[0:64, 0:255], W2[:, hc, :], hT[g * 2 + hc][:, 0:255], start=(hc == 0), stop=(hc == 1),
                                   r=['W2', ('hT', g * 2 + hc)], w=['pk2'])
                            ts('dve', k1[:, 0:255], pk2[0:64, 0:255], b2c[:, 0:1], ALU.add, r=['pk2', 'b2c'], w=['k1'])
                            cp('dve', kx[:, 0:255], k1[:, 0:255], r=['k1'], w=['kx'])
                            mm(pk2[0:64, 256:511], Rm[0:64, 0:64], kx[:, 0:255], r=['Rm', 'kx'], w=['pk2'])
                            tt('dve', k2[:, 0:255], pk2[0:64, 256:511], cC[:, 1, 0:255], ALU.mult, r=['pk2', 'cC1'], w=['k2'])
                            tt('dve', k1[:, 0:255], k1[:, 0:255], cC[:, 0, 0:255], ALU.mult, r=['k1', 'cC0'], w=['k1'])
                            tt('dve', KCT[g][:, 0:255], k1[:, 0:255], k2[:, 0:255], ALU.add, r=['k1', 'k2'], w=[('KCT', g)])
                        else:
                            for ct in range(2):
                                M = 128 if ct == 0 else 127
                                for hc in range(2):
                                    mm(pk2[0:M, ct * 64:(ct + 1) * 64], hT[g * 2 + hc][:, ct * 128:ct * 128 + M], W2[:, hc, :],
                                       start=(hc == 0), stop=(hc == 1), r=['W2', ('hT', g * 2 + hc)], w=['pk2'])
                                tt('dve', VOZ[g][0:M, ct, 0:64], pk2[0:M, ct * 64:(ct + 1) * 64], b2r[0:M, :], ALU.add,
                                   r=['pk2', 'b2r'], w=[('VOZ', g)])
                dma(vis, I['vis'], w=['vis'])
                dma(validm, I['validm'], w=['validm'])
                dma(bonus, I['bonus'], w=['bonus'])
                for g in range(2):
                    dma(VOZ[g][:, :, 64:128], I['ovl'], w=[('VOZo', g)])
                    mset('pool', VOZ[g][:, :, 128:129], 1.0, w=[('VOZ1', g)])
            S.barrier()
            QA = [sbt(es, f"QA{i}", [128, 4, 128], BF16) for i in range(2)]
            pcm = [sbt(es, f"pcm{i}", [128, 512], BF16) for i in range(2)]
            pt = [sbt(es, f"ptn{i}", [128, 512], BF16) for i in range(3)]
            osb = sbt(es, "osb3", [65, 512], F32)
            rz = sbt(es, "rz3", [128, 4], F32)
            rzA = sbt(es, "rzA", [128, 8], F32)
            ocmp = [sbt(es, f"ocmp{i}", [128, 4, 64], F32) for i in range(3)]
            oslc = sbt(es, "oslc", [128, 4, 64], F32)
            owin = [sbt(es, f"owin{i}", [128, 4, 64], F32) for i in range(2)]
            pendB = []
            pendN = []
            onsa = [sbt(es, f"onsa{i}", [128, 4, 64], F32) for i in range(2)]
            impw = sbt(es, "impw", [128, 4, 64], F32)
            score_t = sbt(es, "score", [128, 64], F32)
            tmpm = sbt(es, "tmpm", [128, 64], F32)
            m8 = sbt(es, "m8", [128, 16], F32)
            negm = sbt(es, "negm", [128, 128], BF16)
            pS = [pst(es, f"pS3{i}") for i in range(3)]
            pO = [pst(es, f"pO3{i}") for i in range(3)]
            pcz = [pst(es, f"pcz{i}") for i in range(2)]
            mset('pool', negm, 0.0, w=['negm'])
            vk = [('VOZ', 0), ('VOZ', 1), ('VOZo', 0), ('VOZo', 1), ('VOZ1', 0), ('VOZ1', 1)]
            items = [(g, qt) for g in range(2) for qt in range(LIM.get('p3q', 32))]

            def ctx(i):
                g, qt = items[i]
                s2 = i % 2
                qa = QA[s2]
                return g, qt, s2, qa, qa.rearrange("p h q -> p (h q)"), gates[:, qt, g * 12:(g + 1) * 12].rearrange("p (h b) -> p h b", b=3)

            def stageA1a(i):
                g, qt, s2, qa, qa2, gsl = ctx(i)
                dma(qa[0:64, :, :], FM[1024 + g * 256:1024 + (g + 1) * 256, qt * 128:(qt + 1) * 128].rearrange("(h d) q -> d h q", h=4),
                    r=['FM'], w=[('qa', s2, 'q')])
                nct = 1 if qt < 16 else 2
                for ct in range(nct):
                    M = 128 if ct == 0 else 127
                    mm(pcz[ct][0:M, :], KCT[g][:, ct * 128:ct * 128 + M], qa2[0:64, :], r=[('KCT', g), ('qa', s2, 'q')], w=[('pcz', ct)])
                    act(pcm[ct][0:M, :], pcz[ct][0:M, :], AF.Exp, scale=0.125, r=[('pcz', ct)], w=[('pcm', ct)])
                    tt('pool', pcm[ct][0:M, :].rearrange("p (h q) -> p h q", h=4), pcm[ct][0:M, :].rearrange("p (h q) -> p h q", h=4),
                       vis[0:M, ct, qt * 128:(qt + 1) * 128].unsqueeze(1).broadcast_to([M, 4, 128]), ALU.mult,
                       r=[('pcm', ct), 'vis'], w=[('pcm', ct)])

            def stageA1b(i):
                g, qt, s2, qa, qa2, gsl = ctx(i)
                nct = 1 if qt < 16 else 2
                oc = ocmp[i % 3]
                for h in range(4):
                    pz = pcz[h // 2]
                    for ct in range(nct):
                        M = 128 if ct == 0 else 127
                        mm(pz[:, (h % 2) * 256:(h % 2) * 256 + 129], pcm[ct][0:M, h * 128:(h + 1) * 128], VOZ[g][0:M, ct, :],
                           start=(ct == 0), stop=(ct == nct - 1), r=[('pcm', 0), ('pcm', 1)] + vk, w=[('pcz', h // 2)])
                for hb in range(2):
                    pzv = pcz[hb].rearrange("p (h c) -> p h c", h=2)
                    ts('dve', rzA[:, 2 * hb:2 * hb + 2], pzv[:, :, 128], 1e-30, ALU.max, r=[('pcz', hb)], w=['rzA'])
                recip(rzA[:, 0:4], rzA[:, 0:4], r=['rzA'], w=['rzA'])
                for hb in range(2):
                    pzv = pcz[hb].rearrange("p (h c) -> p h c", h=2)
                    tt('dve', impw[:, 2 * hb:2 * hb + 2, :], pzv[:, :, 64:128], rzA[:, 2 * hb:2 * hb + 2].unsqueeze(2).broadcast_to([128, 2, 64]),
                       ALU.mult, r=[('pcz', hb), 'rzA'], w=['impw'])
                tt('dve', rzA[:, 4:8], rzA[:, 0:4], gsl[:, :, 0], ALU.mult, r=['rzA', 'gates'], w=['rzA2'])
                for hb in range(2):
                    pzv = pcz[hb].rearrange("p (h c) -> p h c", h=2)
                    tt('dve', oc[:, 2 * hb:2 * hb + 2, :], pzv[:, :, 0:64], rzA[:, 4 + 2 * hb:6 + 2 * hb].unsqueeze(2).broadcast_to([128, 2, 64]),
                       ALU.mult, r=[('pcz', hb), 'rzA2'], w=[('ocmp', i % 3)])
                red('dve', score_t, impw.rearrange("p h j -> p j h"), ALU.add, r=['impw'], w=['score'])
                tt('dve', score_t, score_t, validm[:, qt, :], ALU.mult, r=['score', 'validm'], w=['score'])
                tt('dve', score_t, score_t, bonus[:, qt, :], ALU.add, r=['score', 'bonus'], w=['score'])
                S.op('dve', lambda e: e.max(out=m8[:, 0:8], in_=score_t), reads=['score'], writes=['m8'])
                S.op('dve', lambda e: e.match_replace(out=tmpm, in_to_replace=m8[:, 0:8], in_values=score_t, imm_value=-2.0),
                     reads=['score', 'm8'], writes=['tmpm'])
                S.op('dve', lambda e: e.max(out=m8[:, 8:16], in_=tmpm), reads=['tmpm'], writes=['m8b'])
                ts('dve', negm[:, 64:128], score_t, m8[:, 15:16], ALU.is_lt, NEGM, ALU.mult, r=['score', 'm8b'], w=['negm'])

            def stageA2(i):
                g, qt, s2, qa, qa2, gsl = ctx(i)
                pT = pcz[0].bitcast(BF16)
                tp(pT[:, 0:128], negm, identb, r=['negm', 'identb'], w=[('pcz', 0)])
                cp('dve', qa[64:128, :, :], pT[64:128, 0:128].unsqueeze(1).broadcast_to([64, 4, 128]), r=[('pcz', 0)], w=[('qa', s2, 'm')])

            def stageB(i):
                g, qt, s2, qa, qa2, gsl = ctx(i)
                qq = [('qa', s2, 'q')]
                qm = [('qa', s2, 'q'), ('qa', s2, 'm')]
                tiles = []
                w0 = max(0, qt - 4)
                for kt in range(w0, qt + 1):
                    tiles.append(('w', kt, kt == w0, kt == qt))
                for kt in range(qt + 1):
                    tiles.append(('s', kt, kt == 0, kt == qt))

                def score(n):
                    br, kt, first, last = tiles[n]
                    if br == 's':
                        mm(pS[n % 3], KSA[g][:, kt * 128:(kt + 1) * 128], qa2, r=[('KSA', g), ('KSAe', g)] + qm, w=[('pS', n % 3)])
                    else:
                        mm(pS[n % 3], KW[g][:, kt * 128:(kt + 1) * 128], qa2[0:64, :], r=[('KW', g)] + qq, w=[('pS', n % 3)])
                score(0)
                if len(tiles) > 1:
                    score(1)
                for n in range(len(tiles)):
                    br, kt, first, last = tiles[n]
                    if n + 2 < len(tiles):
                        score(n + 2)
                    if n == 1 and i + 1 < len(items):
                        stageA1b(i + 1)
                    if n == min(3, len(tiles) - 1) and pendN:
                        pendN.pop(0)()
                    b3 = n % 3
                    act(pt[b3], pS[b3], AF.Exp, scale=0.125, r=[('pS', b3)], w=[('pt', b3)])
                    p3 = pt[b3].rearrange("p (h q) -> p h q", h=4)
                    if kt == qt:
                        tt('pool', p3, p3, tri.unsqueeze(1).broadcast_to([128, 4, 128]), ALU.mult, r=[('pt', b3), 'tri'], w=[('pt', b3)])
                    if br == 'w' and kt == qt - 4:
                        tt('pool', p3, p3, atri.unsqueeze(1).broadcast_to([128, 4, 128]), ALU.mult, r=[('pt', b3), 'atri'], w=[('pt', b3)])
                    bi = (i % 2) if br == 's' else 2
                    V = VS if br == 's' else VW
                    mm(pO[bi][0:65, :], V[:, kt, g, :], pt[b3], start=first, stop=last, r=['VS', 'VW', ('pt', b3)], w=[('pO', bi)])
                    if last and br == 'w':
                        def fin_w(bi=bi, gsl=gsl, s2=s2):
                            finalize(pO[bi], osb, [('pO', bi), 'osb'], pO[bi], ('pO', bi), rz, owin[s2], ('owin', s2), gate_ap=gsl[:, :, 2])
                        pendB.append((n + 3, fin_w))
                    if last and br == 's':
                        def fin_s(bi=bi, gsl=gsl, s2=s2, g=g, qt=qt, i=i):
                            finalize(pO[bi], osb, [('pO', bi), 'osb'], pO[bi], ('pO', bi), rz, oslc, 'oslc', gate_ap=gsl[:, :, 1])
                            o2 = i % 2
                            tt('dve', onsa[o2], ocmp[i % 3], oslc, ALU.add, r=[('ocmp', i % 3), 'oslc'], w=[('onsa', o2)])
                            tt('dve', onsa[o2], onsa[o2], owin[s2], ALU.add, r=[('onsa', o2), ('owin', s2)], w=[('onsa', o2)])
                            dma(OS[qt * 128:(qt + 1) * 128, 512 + g * 256:512 + (g + 1) * 256], onsa[o2].rearrange("p h d -> p (h d)"),
                                r=[('onsa', o2)], w=[('OSn', g, qt)], q='sp')
                        pendN.append(fin_s)
                    while pendB and (pendB[0][0] <= n or n == len(tiles) - 1):
                        pendB.pop(0)[1]()

            stageA1a(0)
            stageA1b(0)
            stageA2(0)
            for i in range(len(items)):
                if i + 1 < len(items):
                    stageA1a(i + 1)
                stageB(i)
                if i + 1 < len(items):
                    stageA2(i + 1)
            while pendN:
                pendN.pop(0)()
        S.barrier()

    if 1 in phases or 2 in phases or 3 in phases:
        esV.close()
    esW = ExitStack()
    PREF = [False]
    if 2 in phases:
        with ExitStack() as es:
            QFA = [sbt(es, f"QFA{i}", [128, S_LEN], BF16) for i in range(2)]
            KFA = [sbt(es, f"KFA{i}", [128, S_LEN], BF16) for i in range(2)]
            pt = [sbt(es, f"pt{i}", [128, 512], BF16) for i in range(4)]
            osb = sbt(es, "osb", [65, 512], F32)
            rz = sbt(es, "rz", [128, 4], F32)
            otok = [sbt(es, f"otok{i}", [128, 4, 64], F32) for i in range(2)]
            pS = [pst(es, f"pS{i}") for i in range(4)]
            pO = [pst(es, f"pO{i}") for i in range(2)]
            ptr = pst(es, "ptr")
            if 4 in phases:
                Wu = nc.alloc_sbuf_tensor_at("sb_Wu", [128, 8 * 4096], BF16, offset=229376 - 64 - 65536).ap().rearrange("p (k n) -> p k n", k=8)
                Wo = nc.alloc_sbuf_tensor_at("sb_Wo", [128, 8 * D], BF16, offset=229376 - 64 - 65536 - 16384).ap().rearrange("p (k n) -> p k n", k=8)
                wstf = [sbt(es, f"wstf{i}", [128, 2048], F32) for i in range(2)]
                PREF[0] = True
                assert nc.sbuf_bytes_remaining >= 81984 + 256, nc.sbuf_bytes_remaining
            for i in range(2):
                mset('pool', QFA[i], 0.0, w=[('qfa', i, 'q'), ('qfa', i, 'h'), ('qfa', i, 'l')])
                mset('pool', KFA[i], 0.0, w=[('kfa', i)])
                mset('pool', KFA[i][64:65, :], 1.0, w=[('kfa', i)])
                mset('pool', KFA[i][96:97, :], 1.0, w=[('kfa', i)])
            oi_box = [0]
            pend = []
            for h in range(LIM.get('p2h', 8)):
                s2 = h % 2
                dma(QFA[s2][0:64, :], FM[h * 64:(h + 1) * 64, :], r=['FM'], w=[('qfa', s2, 'q')])
                dma(QFA[s2][64:65, :], chi[h:h + 1, :], r=['chi'], w=[('qfa', s2, 'h')])
                dma(QFA[s2][96:97, :], clo[h:h + 1, :], r=['clo'], w=[('qfa', s2, 'l')])
                dma(KFA[s2][0:64, :], FM[512 + h * 64:512 + (h + 1) * 64, :], r=['FM'], w=[('kfa', s2)])
                qk = [('qfa', s2, 'q'), ('qfa', s2, 'h'), ('qfa', s2, 'l')]
                if PREF[0]:
                    dma(wstf[0][:, 0:D], I['w_out'][h * 128:(h + 1) * 128, :], w=[('wstf', 0)])
                    cp('pool', Wo[:, h, :], wstf[0][:, 0:D], r=[('wstf', 0)], w=['Wo'])
                    for hf in range(2):
                        dma(wstf[1 - hf], I['w_up'][h * 128:(h + 1) * 128, hf * 2048:(hf + 1) * 2048], w=[('wstf', 1 - hf)])
                        cp('pool', Wu[:, h, hf * 2048:(hf + 1) * 2048], wstf[1 - hf], r=[('wstf', 1 - hf)], w=['Wu'])
                tiles = []
                for qi in range(LIM.get('p2q', 8)):
                    nkt = 4 * qi + 4
                    for kt in range(nkt):
                        j = kt - 4 * qi
                        tiles.append((qi, kt, 128 * j if j > 0 else 0, j >= 0, kt == 0, kt == nkt - 1))

                def score(n):
                    qi, kt, c0, dg, first, last = tiles[n]
                    mm(pS[n % 4][:, c0:512], KFA[s2][:, kt * 128:(kt + 1) * 128], QFA[s2][:, qi * 512 + c0:(qi + 1) * 512],
                       r=qk + [('kfa', s2)], w=[('pS', n % 4)])
                score(0)
                score(1)
                score(2)
                for n in range(len(tiles)):
                    qi, kt, c0, dg, first, last = tiles[n]
                    if n + 3 < len(tiles):
                        score(n + 3)
                    b3 = n % 4
                    act(pt[b3][:, c0:512], pS[b3][:, c0:512], AF.Exp, bias=negc[:, kt, h:h + 1], scale=0.125,
                        r=[('pS', b3), 'negc'], w=[('pt', b3)])
                    if dg:
                        tt('pool', pt[b3][:, c0:c0 + 128], pt[b3][:, c0:c0 + 128], tri, ALU.mult, r=[('pt', b3), 'tri'], w=[('pt', b3)])
                    po = pO[qi % 2]
                    mm(po[0:65, c0:512], VF[:, kt, h, :], pt[b3][:, c0:512], start=first, stop=last,
                       r=['VF', ('pt', b3)], w=[('pO', qi % 2)])
                    if last:
                        def fin_fox(po=po, qi=qi, h=h):
                            o2 = oi_box[0] % 2
                            oi_box[0] += 1
                            finalize(po, osb, [('pO', qi % 2), 'osb'], ptr, 'ptr', rz, otok[o2], ('otok', o2))
                            dma(OS[qi * 512:(qi + 1) * 512, h * 64:(h + 1) * 64].rearrange("(j p) d -> p j d", p=128), otok[o2],
                                r=[('otok', o2)], w=[('OS', h, qi)], q='sp')
                        pend.append((n + 3, fin_fox))
                    while pend and (pend[0][0] <= n or n == len(tiles) - 1):
                        pend.pop(0)[1]()
        S.barrier()

    if 1 in phases or 2 in phases or 3 in phases:
        es12.close()
        S.barrier()

    if 4 in phases:
        with ExitStack() as es:
            if not PREF[0]:
                Wo = sbt(es, "Wo", [128, 8, D], BF16)
                Wu = sbt(es, "Wu", [128, 8, 4096], BF16)
            Wd = sbt(es, "Wd", [128, 32, D], BF16)
            gfn = sbt(es, "gfn", [128, D], F32)
            gml = sbt(es, "gml", [128, D], F32)
            gfi = sbt(es, "gfi", [128, D], F32)
            with ExitStack() as ew:
                wst = [sbt(ew, f"wst4{i}", [128, 2048], F32) for i in range(2)]
                assert (not PREF[0]) or nc.sbuf_bytes_remaining >= 81984 + 256, nc.sbuf_bytes_remaining
                wi = 0
                for kc in range(0 if PREF[0] else 8):
                    st = wst[wi % 2]
                    dma(st[:, 0:D], I['w_out'][kc * 128:(kc + 1) * 128, :], w=[('wst', wi % 2)])
                    cp('pool', Wo[:, kc, :], st[:, 0:D], r=[('wst', wi % 2)], w=['Wo'])
                    wi += 1
                for kc in range(0 if PREF[0] else 8):
                    for hf in range(2):
                        st = wst[wi % 2]
                        dma(st, I['w_up'][kc * 128:(kc + 1) * 128, hf * 2048:(hf + 1) * 2048], w=[('wst', wi % 2)])
                        cp('pool' if hf == 0 else 'dve', Wu[:, kc, hf * 2048:(hf + 1) * 2048], st, r=[('wst', wi % 2)], w=['Wu'])
                        wi += 1
                for hc in range(0, 32, 2):
                    st = wst[wi % 2]
                    dma(st.rearrange("p (c d) -> p c d", c=2), I['w_down'][hc * 128:(hc + 2) * 128, :].rearrange("(c p) d -> p c d", p=128),
                        w=[('wst', wi % 2)])
                    cp('pool' if (hc // 2) % 2 == 0 else 'dve', Wd[:, hc:hc + 2, :], st.rearrange("p (c d) -> p c d", c=2), r=[('wst', wi % 2)], w=['Wd'])
                    wi += 1
                dma(gfn[:, 0:512], I['g_fox'].partition_broadcast(128), w=['gfn0'])
                dma(gfn[:, 512:1024], I['g_nsa'].partition_broadcast(128), w=['gfn1'])
                dma(gml, I['g_mlp'].partition_broadcast(128), w=['gml'])
                dma(gfi, I['g_final'].partition_broadcast(128), w=['gfi'])
            S.barrier()
            xh = [sbt(es, f"xh{i}", [128, D], F32) for i in range(2)]
            ot = [sbt(es, f"ot{i}", [128, D], F32) for i in range(2)]
            sq = sbt(es, "sq4", [128, D], F32)
            ss = sbt(es, "ss4", [128, 2], F32)
            rstd = sbt(es, "rstd4", [128, 2], F32)
            ssm = sbt(es, "ssm4", [128, 2], F32)
            rstdm = sbt(es, "rstdm4", [128, 2], F32)
            yb = sbt(es, "yb", [128, D], BF16)
            ya = sbt(es, "ya", [128, D], BF16)
            yT = sbt(es, "yT", [128, 8, 128], BF16)
            h1T = [sbt(es, f"h1T{i}", [128, 8, 128], BF16) for i in range(2)]
            rl = [sbt(es, f"rl{i}", [128, 128], F32) for i in range(2)]
            hid = sbt(es, "hid", [128, 32, 128], BF16)
            fin = [sbt(es, f"fin{i}", [128, D], F32) for i in range(2)]
            psT = pst(es, "psT4", BF16)
            pP = [pst(es, f"pP{i}") for i in range(2)]
            pA = [pst(es, f"pA{i}") for i in range(2)]
            pU = [pst(es, f"pU{i}") for i in range(3)]
            assert (not PREF[0]) or nc.sbuf_bytes_remaining >= 81984 + 256, nc.sbuf_bytes_remaining
            LIM['_rem4'] = nc.sbuf_bytes_remaining

            def p4load_o(kt):
                dma(ot[kt % 2], OS[kt * 128:(kt + 1) * 128, :], r=['OS'], w=[('ot', kt % 2)])

            def p4load_x(kt):
                dma(xh[kt % 2], I['x'][kt * 128:(kt + 1) * 128, :], w=[('xh', kt % 2)])

            def P_a(kt):
                s2 = kt % 2
                for gi in range(2):
                    tt('dve', sq[:, gi * 512:(gi + 1) * 512], ot[s2][:, gi * 512:(gi + 1) * 512], ot[s2][:, gi * 512:(gi + 1) * 512], ALU.mult,
                       r=[('ot', s2)], w=['sq'])
                red('dve', ss, sq.rearrange("p (g d) -> p g d", g=2), ALU.add, r=['sq'], w=['ss'])
                act(rstd, ss, AF.Ln, bias=epsb[:, 0:1], scale=1.0 / 512, r=['ss', 'epsb'], w=['rstd'])
                act(rstd, rstd, AF.Exp, scale=-0.5, r=['rstd'], w=['rstd'])
                for gi in range(2):
                    stt('dve', ya[:, gi * 512:(gi + 1) * 512], ot[s2][:, gi * 512:(gi + 1) * 512], rstd[:, gi:gi + 1], gfn[:, gi * 512:(gi + 1) * 512],
                        ALU.mult, ALU.mult, r=[('ot', s2), 'rstd', 'gfn0', 'gfn1'], w=['ya'])
                if kt + 2 < NT:
                    p4load_o(kt + 2)

            def P_a2(kt):
                for kc in range(8):
                    tp(psT[:, kc * 128:(kc + 1) * 128], ya[:, kc * 128:(kc + 1) * 128], identb, r=['ya', 'identb'], w=['psT'])
                cp('act', yT, psT.rearrange("p (k t) -> p k t", k=8), r=['psT'], w=['yT'])

            def P_b(kt):
                s2 = kt % 2
                for hf in range(2):
                    for kc in range(8):
                        mm(pP[hf], yT[:, kc, :], Wo[:, kc, hf * 512:(hf + 1) * 512], start=(kc == 0), stop=(kc == 7), r=['yT', 'Wo'], w=[('pP', hf)])
                    tt('dve', xh[s2][:, hf * 512:(hf + 1) * 512], pP[hf], xh[s2][:, hf * 512:(hf + 1) * 512], ALU.add,
                       r=[('pP', hf), ('xh', s2)], w=[('xh', s2)])
                tt('dve', sq, xh[s2], xh[s2], ALU.mult, r=[('xh', s2)], w=['sq'])
                red('dve', ss[:, 0:1], sq, ALU.add, r=['sq'], w=['ss'])
                act(rstd[:, 0:1], ss[:, 0:1], AF.Ln, bias=epsb[:, 0:1], scale=1.0 / D, r=['ss', 'epsb'], w=['rstd'])
                act(rstd[:, 0:1], rstd[:, 0:1], AF.Exp, scale=-0.5, r=['rstd'], w=['rstd'])
                stt('dve', yb, xh[s2], rstd[:, 0:1], gml, ALU.mult, ALU.mult, r=[('xh', s2), 'rstd', 'gml'], w=['yb'])

            def P_c(kt):
                s2 = kt % 2
                for kc in range(8):
                    tp(psT[:, kc * 128:(kc + 1) * 128], yb[:, kc * 128:(kc + 1) * 128], identb, r=['yb', 'identb'], w=['psT'])
                cp('act', h1T[s2], psT.rearrange("p (k t) -> p k t", k=8), r=['psT'], w=[('h1T', s2)])

            p4load_o(0)
            p4load_x(0)
            if NT > 1:
                p4load_o(1)
                p4load_x(1)
            P_a(0)
            P_a2(0)
            if NT > 1:
                P_a(1)
            P_b(0)
            P_c(0)
            for kt in range(NT):
                s2 = kt % 2
                nxt = kt + 1 < NT
                for hc in range(32):
                    pu = pU[hc % 3]
                    for kc in range(8):
                        mm(pu[:, 0:128], Wu[:, kc, hc * 128:(hc + 1) * 128], h1T[s2][:, kc, :], start=(kc == 0), stop=(kc == 7),
                           r=['Wu', ('h1T', s2)], w=[('pU', hc % 3)])
                    act(rl[hc % 2], pu[:, 0:128], AF.Relu, r=[('pU', hc % 3)], w=[('rl', hc % 2)])
                    tt('pool', hid[:, hc, :], rl[hc % 2], rl[hc % 2], ALU.mult, r=[('rl', hc % 2)], w=['hid'])
                    if nxt and hc == 3:
                        P_a2(kt + 1)
                    if nxt and hc == 6:
                        P_b(kt + 1)
                    if kt + 2 < NT and hc == 16:
                        P_a(kt + 2)
                    if nxt and hc == 26:
                        P_c(kt + 1)
                for hf in range(2):
                    for hc in range(32):
                        mm(pA[hf], hid[:, hc, :], Wd[:, hc, hf * 512:(hf + 1) * 512], start=(hc == 0), stop=(hc == 31), r=['hid', 'Wd'], w=[('pA', hf)])
                    tt('dve', xh[s2][:, hf * 512:(hf + 1) * 512], pA[hf], xh[s2][:, hf * 512:(hf + 1) * 512], ALU.add,
                       r=[('pA', hf), ('xh', s2)], w=[('xh', s2)])
                tt('dve', sq, xh[s2], xh[s2], ALU.mult, r=[('xh', s2)], w=['sq'])
                red('dve', ssm[:, 0:1], sq, ALU.add, r=['sq'], w=['ssm'])
                act(rstdm[:, 0:1], ssm[:, 0:1], AF.Ln, bias=epsb[:, 0:1], scale=1.0 / D, r=['ssm', 'epsb'], w=['rstdm'])
                act(rstdm[:, 0:1], rstdm[:, 0:1], AF.Exp, scale=-0.5, r=['rstdm'], w=['rstdm'])
                stt('dve', fin[s2], xh[s2], rstdm[:, 0:1], gfi, ALU.mult, ALU.mult, r=[('xh', s2), 'rstdm', 'gfi'], w=[('fin', s2)])
                dma(out_d[kt * 128:(kt + 1) * 128, :], fin[s2], r=[('fin', s2)], w=[('out', kt)])
                if kt + 2 < NT:
                    p4load_x(kt + 2)
    esW.close()
    S.finish()
    LIM['_cnt'] = dict(S.cnt)
    LIM['_dma'] = {q: list(v) for q, v in S.dma_use.items()}
    S.emit()
    es_all.close()
    return nc


_CONSTS = None


def make_in_maps(inputs):
    global _CONSTS
    if _CONSTS is None:
        _CONSTS = _consts()
    c = _CONSTS
    f = lambda a: np.ascontiguousarray(np.asarray(a, dtype=np.float32))
    shared = {
        'w_in': f(inputs['w_in'][0]), 'w_out': f(inputs['w_out'][0]), 'w_up': f(inputs['w_up'][0]), 'w_down': f(inputs['w_down'][0]),
        'g_attn': f(inputs['g_attn']).reshape(1, D), 'g_mlp': f(inputs['g_mlp']).reshape(1, D), 'g_final': f(inputs['g_final']).reshape(1, D),
        'g_fox': f(inputs['g_fox']).reshape(1, 512), 'g_nsa': f(inputs['g_nsa']).reshape(1, 512),
        'b_f': f(inputs['b_f']).reshape(1, 8), 'b_gate': f(inputs['b_gate']).reshape(1, 24),
        'cmpk_peT': f(np.asarray(inputs['cmpk_pe'][0]).T), 'cmpk_w1': f(inputs['cmpk_w1'][0]),
        'cmpk_b1': f(np.asarray(inputs['cmpk_b1'][0]).reshape(2, 128).T), 'cmpk_w2': f(inputs['cmpk_w2'][0]),
        'cmpk_b2': f(inputs['cmpk_b2'][0]).reshape(64, 1),
        'cmpv_peT': f(np.asarray(inputs['cmpv_pe'][0]).T), 'cmpv_w1': f(inputs['cmpv_w1'][0]),
        'cmpv_b1': f(np.asarray(inputs['cmpv_b1'][0]).reshape(2, 128).T), 'cmpv_w2': f(inputs['cmpv_w2'][0]),
        'cmpv_b2': f(inputs['cmpv_b2'][0]).reshape(1, 64),
    }
    shared.update(c)
    x = np.asarray(inputs['x'], dtype=np.float32)
    return [dict(shared, x=np.ascontiguousarray(x[b])) for b in range(x.shape[0])]


def kernel(**inputs):
    nc = build()
    in_maps = make_in_maps(inputs)
    res = run_bass_kernel_spmd(nc, in_maps, core_ids=list(range(8)))
    return np.stack([np.asarray(r['out'], dtype=np.float32) for r in res.results], axis=0)
```

```python
from contextlib import ExitStack
import numpy as np
import ml_dtypes
import concourse.bass as bass
import concourse.mybir as mybir
from concourse.bass_utils import run_bass_kernel_spmd

F32 = mybir.dt.float32
BF16 = mybir.dt.bfloat16
AF = mybir.ActivationFunctionType
ALU = mybir.AluOpType
AX = mybir.AxisListType
bf = ml_dtypes.bfloat16

S_LEN = 4096
D = 1024
NT = 32
EPS = 1e-6
NEGM = -30000.0
LIM = {}


class Sched:
    ENG = ('pe', 'act', 'dve', 'pool', 'sp')
    DMAQ = ('sp', 'act', 'pool')

    def __init__(self, nc, ndma=8, selfsync=True):
        self.nc = nc
        self.selfsync = selfsync
        self.semh = {}
        for e in self.ENG:
            self.semh['s_' + e] = nc.alloc_semaphore(name='s_' + e)
        self.ndma = {q: (ndma if q == 'sp' else 8) for q in self.DMAQ}
        for q in self.DMAQ:
            for i in range(self.ndma[q]):
                self.semh[f'd_{q}{i}'] = nc.alloc_semaphore(name=f'd_{q}{i}')
        self.cnt = {e: 0 for e in self.ENG}
        self.prog = {e: [] for e in self.ENG}
        self.seen = {e: {} for e in self.ENG}
        self.bufs = {}
        self.dma_use = {q: [0] * self.ndma[q] for q in self.DMAQ}
        self.dma_rr = {q: 0 for q in self.DMAQ}

    def _deps(self, eng, reads, writes):
        toks = []
        for b in reads:
            st = self.bufs.get(b)
            if st and st[0]:
                toks.append(st[0])
        for b in writes:
            st = self.bufs.get(b)
            if st:
                if st[0]:
                    toks.append(st[0])
                toks.extend(st[1])
        best = {}
        for (sk, val, teng) in toks:
            if teng == eng and (eng == 'pe' or not self.selfsync):
                continue
            if self.seen[eng].get(sk, 0) >= val:
                continue
            if best.get(sk, 0) < val:
                best[sk] = val
        for sk, val in best.items():
            self.seen[eng][sk] = val
        return list(best.items())

    def _mark(self, tok, reads, writes):
        for b in reads:
            self.bufs.setdefault(b, [None, []])[1].append(tok)
        for b in writes:
            self.bufs[b] = [tok, []]

    PSK = {'psT', 'pfm', 'prr', 'ptA', 'ptB', 'pcc', 'pS', 'pO', 'ptr', 'ph', 'pk2', 'pb1', 'pcz', 'pA', 'pU', 'pP'}

    def op(self, eng, fn, reads=(), writes=()):
        ex = [b for b in reads if (b if isinstance(b, str) else b[0]) in self.PSK]
        if ex:
            reads = [b for b in reads if b not in ex]
            writes = list(writes) + ex
        waits = self._deps(eng, reads, writes)
        self.cnt[eng] += 1
        tok = ('s_' + eng, self.cnt[eng], eng)
        self.prog[eng].append(('op', waits, fn))
        self._mark(tok, reads, writes)

    def dma(self, q, out, in_, reads=(), writes=()):
        i = self.dma_rr[q]
        self.dma_rr[q] = (i + 1) % self.ndma[q]
        sk = f'd_{q}{i}'
        prev = self.dma_use[q][i]
        waits = self._deps(q, reads, writes)
        if prev > 0 and self.seen[q].get(sk, 0) < 16 * prev:
            self.seen[q][sk] = 16 * prev
            waits = [w for w in waits if w[0] != sk] + [(sk, 16 * prev)]
        self.dma_use[q][i] = prev + 1
        tok = (sk, 16 * (prev + 1), 'dma')
        self.prog[q].append(('dma', waits, (out, in_, sk)))
        self._mark(tok, reads, writes)

    def _all_waits(self, eng, own=False):
        waits = []
        for e in self.ENG:
            if (e != eng or own) and self.cnt[e] > 0 and self.seen[eng].get('s_' + e, 0) < self.cnt[e]:
                self.seen[eng]['s_' + e] = self.cnt[e]
                waits.append(('s_' + e, self.cnt[e]))
        for q in self.DMAQ:
            for i in range(self.ndma[q]):
                v = 16 * self.dma_use[q][i]
                sk = f'd_{q}{i}'
                if v > 0 and self.seen[eng].get(sk, 0) < v:
                    self.seen[eng][sk] = v
                    waits.append((sk, v))
        return waits

    def barrier(self):
        cnt0 = dict(self.cnt)
        for e in self.ENG:
            w = self._all_waits(e, own=True)
            if w:
                self.prog[e].append(('wait', w, None))
        self.bufs = {}

    def finish(self):
        self.prog['sp'].append(('wait', self._all_waits('sp'), None))

    def emit(self):
        nc = self.nc
        with nc.Block() as block:
            def run(ename):
                def body(eng):
                    for kind, waits, payload in self.prog[ename]:
                        for sk, val in waits:
                            eng.wait_ge(self.semh[sk], val)
                        if kind == 'op':
                            ins = payload(eng)
                            ins.then_inc(self.semh['s_' + ename], 1)
                        elif kind == 'dma':
                            out, in_, sk = payload
                            eng.dma_start(out=out, in_=in_).then_inc(self.semh[sk], 16)
                return body
            block.tensor(run('pe'))
            block.scalar(run('act'))
            block.vector(run('dve'))
            block.gpsimd(run('pool'))
            block.sync(run('sp'))


def _consts():
    c = {}
    c['identb'] = np.eye(128, dtype=np.float32).astype(bf)
    c['identf'] = np.eye(128, dtype=np.float32)
    k = np.arange(128)[:, None]
    q = np.arange(128)[None, :]
    c['tri'] = (k <= q).astype(np.float32).astype(bf)
    c['atri'] = (k > q).astype(np.float32).astype(bf)
    c['U'] = (k <= q).astype(np.float32)
    c['onesf'] = np.ones((128, 128), np.float32)
    R = np.zeros((128, 128), np.float32)
    for m in range(128):
        if m % 64 < 32:
            R[m + 32, m] = -1.0
        else:
            R[m - 32, m] = 1.0
    c['R'] = R.astype(bf)
    half = 32
    inv = (10000.0 ** (-np.arange(half, dtype=np.float32) / half)).astype(np.float32)
    pos = np.arange(S_LEN, dtype=np.float32)
    ang = (pos[None, :] * inv[:, None]).astype(np.float32)
    c['cosT'] = np.tile(np.cos(ang).astype(np.float32), (4, 1))
    c['sinT'] = np.tile(np.sin(ang).astype(np.float32), (4, 1))
    posc = (np.arange(256, dtype=np.float32) * 16 + 31)
    angc = (posc[None, :] * inv[:, None]).astype(np.float32)
    c['cosC'] = np.tile(np.cos(angc).astype(np.float32), (4, 1))
    c['sinC'] = np.tile(np.sin(angc).astype(np.float32), (4, 1))
    s = np.arange(S_LEN)
    c['E'] = (s[None, :] // 64 == np.arange(64)[:, None]).astype(np.float32).astype(bf)
    cc = np.arange(256)
    vis = ((16 * cc[:, None] + 31 <= s[None, :]) & (cc[:, None] < 255)).astype(np.float32)
    c['vis'] = np.ascontiguousarray(vis.reshape(2, 128, S_LEN).transpose(1, 0, 2)).astype(bf)
    j = np.arange(64)
    ovl = ((16 * cc[:, None] < 64 * j[None, :] + 64) & (16 * cc[:, None] + 32 > 64 * j[None, :]) & (cc[:, None] < 255)).astype(np.float32)
    c['ovl'] = np.ascontiguousarray(ovl.reshape(2, 128, 64).transpose(1, 0, 2)).astype(bf)
    cur = s // 64
    valid = (j[None, :] <= cur[:, None])
    forced = (j[None, :] == 0) | (j[None, :] == cur[:, None]) | (j[None, :] == cur[:, None] - 1)
    bonus = np.where(valid, np.where(forced, 1e4, 0.0), -1.0).astype(np.float32)
    c['validm'] = np.ascontiguousarray(valid.astype(np.float32).reshape(32, 128, 64).transpose(1, 0, 2))
    c['bonus'] = np.ascontiguousarray(bonus.reshape(32, 128, 64).transpose(1, 0, 2))
    return c


IN_SPECS = [
    ('x', (S_LEN, D), F32), ('w_in', (D, 2848), F32), ('w_out', (D, D), F32), ('w_up', (D, 4096), F32),
    ('w_down', (4096, D), F32), ('g_attn', (1, D), F32), ('g_mlp', (1, D), F32), ('g_final', (1, D), F32),
    ('g_fox', (1, 512), F32), ('g_nsa', (1, 512), F32), ('b_f', (1, 8), F32), ('b_gate', (1, 24), F32),
    ('cmpk_peT', (64, 32), F32), ('cmpk_w1', (2048, 256), F32), ('cmpk_b1', (128, 2), F32), ('cmpk_w2', (256, 64), F32),
    ('cmpk_b2', (64, 1), F32),
    ('cmpv_peT', (64, 32), F32), ('cmpv_w1', (2048, 256), F32), ('cmpv_b1', (128, 2), F32), ('cmpv_w2', (256, 64), F32),
    ('cmpv_b2', (1, 64), F32),
    ('identb', (128, 128), BF16), ('identf', (128, 128), F32), ('tri', (128, 128), BF16), ('atri', (128, 128), BF16),
    ('U', (128, 128), F32), ('onesf', (128, 128), F32), ('R', (128, 128), BF16),
    ('cosT', (128, S_LEN), F32), ('sinT', (128, S_LEN), F32), ('cosC', (128, 256), F32), ('sinC', (128, 256), F32),
    ('E', (64, S_LEN), BF16), ('vis', (128, 2, S_LEN), BF16), ('ovl', (128, 2, 64), BF16),
    ('validm', (128, 32, 64), F32), ('bonus', (128, 32, 64), F32),
]


def build(phases=(1, 2, 3, 4), dbg=False):
    nc = bass.Bass("TRN2", target_bir_lowering=False)
    I = {}
    for name, shape, dt in IN_SPECS:
        I[name] = nc.dram_tensor(name, list(shape), dt, kind="ExternalInput").ap()
    out_d = nc.dram_tensor("out", [S_LEN, D], F32, kind="ExternalOutput").ap()
    skind = "ExternalOutput" if dbg else "Internal"
    FM = nc.dram_tensor("FM", [2048, S_LEN], BF16, kind=skind).ap()
    OS = nc.dram_tensor("OS", [S_LEN, D], F32, kind=skind).ap()
    DBG = nc.dram_tensor("DBG", [128, 2048], F32, kind=skind).ap()

    S = Sched(nc, ndma=LIM.get("ndma", 40))
    es_all = ExitStack()

    def sbt(es, name, shape, dt):
        return es.enter_context(nc.sbuf_tensor("sb_" + name, list(shape), dt)).ap()

    def pst(es, name, dt=F32):
        return es.enter_context(nc.psum_tensor("ps_" + name, [128, 2048 // (4 if dt == F32 else 2)], dt)).ap()

    def mm(out, lhsT, rhs, start=True, stop=True, r=(), w=()):
        S.op('pe', lambda e: e.matmul(out, lhsT=lhsT, rhs=rhs, start=start, stop=stop), reads=r, writes=w)

    def tp(out, in_, ident, r=(), w=()):
        S.op('pe', lambda e: e.transpose(out=out, in_=in_, identity=ident), reads=r, writes=w)

    def act(out, in_, func, bias=0.0, scale=1.0, r=(), w=()):
        S.op('act', lambda e: e.activation(out=out, in_=in_, func=func, bias=bias, scale=scale), reads=r, writes=w)

    def cp(eng, out, in_, r=(), w=()):
        if eng == 'act':
            S.op('act', lambda e: e.copy(out=out, in_=in_), reads=r, writes=w)
        else:
            S.op(eng, lambda e: e.tensor_copy(out=out, in_=in_), reads=r, writes=w)

    def tt(eng, out, in0, in1, op, r=(), w=()):
        S.op(eng, lambda e: e.tensor_tensor(out=out, in0=in0, in1=in1, op=op), reads=r, writes=w)

    def ts(eng, out, in0, s1, op0, s2=None, op1=None, r=(), w=()):
        if op1 is None:
            S.op(eng, lambda e: e.tensor_scalar(out=out, in0=in0, scalar1=s1, scalar2=None, op0=op0), reads=r, writes=w)
        else:
            S.op(eng, lambda e: e.tensor_scalar(out=out, in0=in0, scalar1=s1, scalar2=s2, op0=op0, op1=op1), reads=r, writes=w)

    def stt(eng, out, in0, sc, in1, op0, op1, r=(), w=()):
        S.op(eng, lambda e: e.scalar_tensor_tensor(out=out, in0=in0, scalar=sc, in1=in1, op0=op0, op1=op1), reads=r, writes=w)

    def red(eng, out, in_, op, r=(), w=()):
        S.op(eng, lambda e: e.tensor_reduce(out=out, in_=in_, axis=AX.X, op=op), reads=r, writes=w)

    def recip(out, in_, r=(), w=()):
        S.op('dve', lambda e: e.reciprocal(out=out, in_=in_), reads=r, writes=w)

    def mset(eng, ap, val, w=()):
        S.op(eng, lambda e: e.memset(ap, val), writes=w)

    def dma(out, in_, r=(), w=(), q='sp'):
        S.dma(q, out, in_, reads=r, writes=w)

    def rmsstat(x_ap, n, sq, ss, rstd, rk, tag):
        tt('dve', sq, x_ap, x_ap, ALU.mult, r=rk, w=[tag + 'sq'])
        red('dve', ss, sq, ALU.add, r=[tag + 'sq'], w=[tag + 'ss'])
        act(rstd, ss, AF.Ln, bias=epsb[:, 0:1], scale=1.0 / n, r=[tag + 'ss', 'epsb'], w=[tag + 'rstd'])
        act(rstd, rstd, AF.Exp, scale=-0.5, r=[tag + 'rstd'], w=[tag + 'rstd'])

    P = es_all
    identb = sbt(P, "identb", [128, 128], BF16)
    identf = sbt(P, "identf", [128, 128], F32)
    tri = sbt(P, "tri", [128, 128], BF16)
    atri = sbt(P, "atri", [128, 128], BF16)
    epsb = sbt(P, "epsb", [128, 2], F32)
    for nm, t in (('identb', identb), ('identf', identf), ('tri', tri), ('atri', atri)):
        dma(t, I[nm], w=[nm])
    mset('dve', epsb[:, 0:1], EPS, w=['epsb'])
    mset('dve', epsb[:, 1:2], 1.0, w=['epsb'])

    if 1 in phases or 2 in phases or 3 in phases:
        es12 = ExitStack()
        VF = sbt(es12, "VF", [128, NT, 8, 65], BF16)
        logf = sbt(es12, "logf", [128, NT, 8], F32)
        gates = sbt(es12, "gates", [128, NT, 24], F32)
        negc = sbt(es12, "negc", [128, NT, 8], F32)
        chi = sbt(es12, "chi", [8, S_LEN], BF16)
        clo = sbt(es12, "clo", [8, S_LEN], BF16)
        esV = ExitStack()
        VS = sbt(esV, "VS", [128, NT, 2, 65], BF16)
        VW = sbt(esV, "VW", [128, NT, 2, 65], BF16)
        mset('pool', logf.rearrange("p a b -> p (a b)"), 0.0, w=['logf'])
        mset('pool', gates.rearrange("p a b -> p (a b)"), 0.0, w=['gates'])
        mset('pool', VF.rearrange("p a b c -> p (a b c)"), 1.0, w=['VF'])
        mset('pool', VS.rearrange("p a b c -> p (a b c)"), 1.0, w=['VS'])
        mset('pool', VW.rearrange("p a b c -> p (a b c)"), 1.0, w=['VW'])

    if 1 in phases:
        with ExitStack() as es:
            Wfm = sbt(es, "Wfm", [128, 8, 2048], BF16)
            Wtm = sbt(es, "Wtm", [128, 8, 800], BF16)
            wst = [sbt(es, f"wst{i}", [128, 2848], F32) for i in range(2)]
            gat = sbt(es, "gat", [128, D], F32)
            bfb = sbt(es, "bfb", [128, 8], F32)
            bgb = sbt(es, "bgb", [128, 24], F32)
            Rm = sbt(es, "Rm", [128, 128], BF16)
            Um = sbt(es, "Um", [128, 128], F32)
            On = sbt(es, "On", [128, 128], F32)
            xt = [sbt(es, f"xt{i}", [128, D], F32) for i in range(2)]
            sq = sbt(es, "sq", [128, D], F32)
            ss = sbt(es, "ss", [128, 1], F32)
            rstd = sbt(es, "rstd", [128, 1], F32)
            hnb = [sbt(es, f"hnb{i}", [128, D], BF16) for i in range(2)]
            hnT = [sbt(es, f"hnT{i}", [128, 8, 512], BF16) for i in range(2)]
            fmo = [sbt(es, f"fmo{i}", [128, 512], BF16) for i in range(4)]
            cs = [sbt(es, f"cs{i}", [128, 2, 512], F32) for i in range(2)]
            xb = [sbt(es, f"xb{i}", [128, 512], BF16) for i in range(2)]
            t1 = [sbt(es, f"t1{i}", [128, 512], F32) for i in range(2)]
            t2 = [sbt(es, f"t2{i}", [128, 512], F32) for i in range(2)]
            zt = sbt(es, "zt", [128, 32], F32)
            Xp = sbt(es, "Xp", [128, NT, 8], F32)
            psT = pst(es, "psT", BF16)
            pfm = [pst(es, f"pfm{i}") for i in range(2)]
            prr = pst(es, "prr")
            ptA = [pst(es, f"ptA{i}") for i in range(2)]
            ptB = [pst(es, f"ptB{i}") for i in range(2)]
            pcc = ptA[0]

            dma(gat, I['g_attn'].partition_broadcast(128), w=['gat'])
            dma(bfb, I['b_f'].partition_broadcast(128), w=['bfb'])
            dma(bgb, I['b_gate'].partition_broadcast(128), w=['bgb'])
            dma(Rm, I['R'], w=['Rm'])
            dma(Um, I['U'], w=['Um'])
            dma(On, I['onesf'], w=['On'])
            fm_src = [(0, 1024, 0), (1544, 2056, 1024), (2056, 2312, 1536), (2312, 2440, 1792), (2568, 2696, 1920)]
            tm_src = [(1024, 1536, 0), (2440, 2568, 512), (2696, 2824, 640), (1536, 1544, 768), (2824, 2848, 776)]
            pieces = [('Wfm', 0, 512, 0, 'pool'), ('Wfm', 512, 1024, 512, 'dve'), ('Wfm', 1544, 2056, 1024, 'act'),
                      ('Wfm', 2056, 2312, 1536, 'dve'), ('Wfm', 2312, 2440, 1792, 'act'), ('Wfm', 2568, 2696, 1920, 'act'),
                      ('Wtm', 1024, 1536, 0, 'pool'), ('Wtm', 2440, 2568, 512, 'act'), ('Wtm', 2696, 2824, 640, 'dve'),
                      ('Wtm', 1536, 1544, 768, 'dve'), ('Wtm', 2824, 2848, 776, 'dve')]
            for kc in range(8):
                st = wst[kc % 2]
                dma(st, I['w_in'][kc * 128:(kc + 1) * 128, :], w=[('wst', kc % 2)])
                for pi, (wn, a_, b_, d0, eng) in enumerate(pieces):
                    dst = (Wfm if wn == 'Wfm' else Wtm)[:, kc, d0:d0 + (b_ - a_)]
                    S.op(eng, (lambda e, dst=dst, src=st[:, a_:b_]: e.copy(out=dst, in_=src)) if eng == 'act'
                         else (lambda e, dst=dst, src=st[:, a_:b_]: e.tensor_copy(out=dst, in_=src)),
                         reads=[('wst', kc % 2)], writes=[(wn, kc, pi)])
            ROPE = {8, 9, 10, 11, 14, 15}
            NCH = LIM.get('p1', 8)
            cnt = {'fi': 0, 'ri': 0}

            def cs_load(ci):
                c_s = ci % 2
                dma(cs[c_s][:, 0, :], I['cosT'][:, ci * 512:(ci + 1) * 512], w=[('cs', c_s, 0)])
                dma(cs[c_s][:, 1, :], I['sinT'][:, ci * 512:(ci + 1) * 512], w=[('cs', c_s, 1)])

            def prologue_tile(ci, t4):
                c_s = ci % 2
                kt = ci * 4 + t4
                s2 = kt % 2
                dma(xt[s2], I['x'][kt * 128:(kt + 1) * 128, :], w=[('xt', s2)])
                rmsstat(xt[s2], D, sq, ss, rstd, [('xt', s2)], 'p1')
                stt('dve', hnb[s2], xt[s2], rstd[:, 0:1], gat, ALU.mult, ALU.mult, r=[('xt', s2), 'p1rstd', 'gat'], w=[('hnb', s2)])
                for kc in range(8):
                    tp(psT[:, kc * 128:(kc + 1) * 128], hnb[s2][:, kc * 128:(kc + 1) * 128], identb, r=[('hnb', s2), 'identb'], w=['psT'])
                cp('act', hnT[c_s][:, :, t4 * 128:(t4 + 1) * 128], psT.rearrange("p (k t) -> p k t", k=8), r=['psT'], w=[('hnT', c_s)])

            if NCH > 0:
                cs_load(0)
                for t4 in range(4):
                    prologue_tile(0, t4)
            S.barrier()
            for ci in range(NCH):
                c_s = ci % 2
                if ci + 1 < NCH:
                    cs_load(ci + 1)
                pend_rot = []

                def rot_part2(pb_unused, r2, fo, fk, mt, ci=ci, c_s=c_s):
                    mm(prr, Rm, xb[r2], r=['Rm', ('xb', r2)], w=['prr'])
                    tt('dve', t2[r2], prr, cs[c_s][:, 1, :], ALU.mult, r=['prr', ('cs', c_s, 1)], w=[('t2', r2)])
                    tt('pool', fo, t1[r2], t2[r2], ALU.add, r=[('t1', r2), ('t2', r2)], w=[fk])
                    dma(FM[mt * 128:(mt + 1) * 128, ci * 512:(ci + 1) * 512], fo, r=[fk], w=[('FM', mt, ci)], q='pool')
                for mt in range(16):
                    pb = pfm[mt % 2]
                    pk = ('pfm', mt % 2)
                    for kc in range(8):
                        mm(pb, Wfm[:, kc, mt * 128:(mt + 1) * 128], hnT[c_s][:, kc, :], start=(kc == 0), stop=(kc == 7),
                           r=['Wfm', ('hnT', c_s)], w=[pk])
                    while pend_rot:
                        rot_part2(*pend_rot.pop(0))
                    fo = fmo[cnt['fi'] % 4]
                    fk = ('fmo', cnt['fi'] % 4)
                    cnt['fi'] += 1
                    if mt not in ROPE:
                        cp('act', fo, pb, r=[pk], w=[fk])
                        dma(FM[mt * 128:(mt + 1) * 128, ci * 512:(ci + 1) * 512], fo, r=[fk], w=[('FM', mt, ci)], q='act')
                    else:
                        r2 = cnt['ri'] % 2
                        cnt['ri'] += 1
                        cp('act', xb[r2], pb, r=[pk], w=[('xb', r2)])
                        tt('dve', t1[r2], pb, cs[c_s][:, 0, :], ALU.mult, r=[pk, ('cs', c_s, 0)], w=[('t1', r2)])
                        pend_rot.append((None, r2, fo, fk, mt))
                    if ci + 1 < NCH and mt in (3, 7, 11, 15):
                        prologue_tile(ci + 1, mt // 4)
                while pend_rot:
                    rot_part2(*pend_rot.pop(0))
                for t4 in range(4):
                    kt = ci * 4 + t4
                    pA_ = ptA[t4 % 2]
                    pB_ = ptB[t4 % 2]
                    ka = ('ptA', t4 % 2)
                    kb = ('ptB', t4 % 2)
                    for kc in range(8):
                        mm(pA_, hnT[c_s][:, kc, t4 * 128:(t4 + 1) * 128], Wtm[:, kc, 0:512], start=(kc == 0), stop=(kc == 7),
                           r=['Wtm', ('hnT', c_s)], w=[ka])
                    for kc in range(8):
                        mm(pB_[:, 0:288], hnT[c_s][:, kc, t4 * 128:(t4 + 1) * 128], Wtm[:, kc, 512:800], start=(kc == 0), stop=(kc == 7),
                           r=['Wtm', ('hnT', c_s)], w=[kb])
                    cp('act', VF[:, kt, :, 0:64], pA_.rearrange("p (h d) -> p h d", h=8), r=[ka], w=['VF'])
                    cp('act', VS[:, kt, :, 0:64], pB_[:, 0:128].rearrange("p (h d) -> p h d", h=2), r=[kb], w=['VS'])
                    cp('act', VW[:, kt, :, 0:64], pB_[:, 128:256].rearrange("p (h d) -> p h d", h=2), r=[kb], w=['VW'])
                    tt('dve', zt[:, 0:8], pB_[:, 256:264], bfb, ALU.add, r=[kb, 'bfb'], w=['zt'])
                    tt('dve', zt[:, 8:32], pB_[:, 264:288], bgb, ALU.add, r=[kb, 'bgb'], w=['zt'])
                    act(zt, zt, AF.Exp, scale=-1.0, r=['zt'], w=['zt'])
                    act(logf[:, kt, :], zt[:, 0:8], AF.Ln, bias=epsb[:, 1:2], r=['zt', 'epsb'], w=['logf'])
                    ts('dve', logf[:, kt, :], logf[:, kt, :], -1.0, ALU.mult, r=['logf'], w=['logf'])
                    ts('dve', zt[:, 8:32], zt[:, 8:32], 1.0, ALU.add, r=['zt'], w=['zt'])
                    recip(gates[:, kt, :], zt[:, 8:32], r=['zt'], w=['gates'])
            mset('dve', Xp[:, 0, :], 0.0, w=['Xp'])
            for kt in range(1, NT if LIM.get('stage', 9) >= 3 else 1):
                tt('dve', Xp[:, kt, :], Xp[:, kt - 1, :], logf[:, kt - 1, :], ALU.add, r=['Xp', 'logf'], w=['Xp'])
            if LIM.get('stage', 9) < 4:
                S.finish(); S.emit(); return nc
            lf2 = logf.rearrange("p k h -> p (k h)")
            xp2 = Xp.rearrange("p k h -> p (k h)")
            mm(pcc[:, 0:256], Um, lf2, start=True, stop=False, r=['Um', 'logf'], w=[('ptA', 0)])
            mm(pcc[:, 0:256], On, xp2, start=False, stop=True, r=['On', 'Xp'], w=[('ptA', 0)])
            ts('dve', negc.rearrange("p k h -> p (k h)"), pcc[:, 0:256], -1.0, ALU.mult, r=[('ptA', 0)], w=['negc'])
            for kg in range(8):
                pb = pfm[kg % 2]
                pk = ('pfm', kg % 2)
                for j4 in range(4):
                    kt = kg * 4 + j4
                    mm(pb[0:8, j4 * 128:(j4 + 1) * 128], logf[:, kt, :], Um, start=True, stop=False, r=['Um', 'logf'], w=[pk])
                    mm(pb[0:8, j4 * 128:(j4 + 1) * 128], Xp[:, kt, :], On, start=False, stop=True, r=['On', 'Xp'], w=[pk])
                ts('dve', chi[:, kg * 512:(kg + 1) * 512], pb[0:8, :], 8.0, ALU.mult, r=[pk], w=['chi'])
                stt('dve', clo[:, kg * 512:(kg + 1) * 512], pb[0:8, :], 8.0, chi[:, kg * 512:(kg + 1) * 512], ALU.mult, ALU.subtract,
                    r=[pk, 'chi'], w=['clo'])
            if dbg:
                dma(DBG[:, 0:256], negc.rearrange("p k h -> p (k h)"), r=['negc'])
                dma(DBG[:, 256:1024], gates.rearrange("p k h -> p (k h)"), r=['gates'])
        S.barrier()

    def finalize(po, osb, osk, ptr, ptrk, rz, otok, otk, gate_ap=None, ev='act'):
        cp(ev, osb[0:65, :], po[0:65, :], r=[osk[0]], w=[osk[1]])
        for j in range(4):
            tp(ptr[:, j * 65:(j + 1) * 65], osb[0:65, j * 128:(j + 1) * 128], identf[0:65, 0:65], r=[osk[1], 'identf'], w=[ptrk])
        pv = ptr[:, 0:260].rearrange("p (j d) -> p j d", j=4)
        recip(rz[:, 0:4], pv[:, :, 64], r=[ptrk], w=['rz'])
        if gate_ap is not None:
            tt('dve', rz[:, 0:4], rz[:, 0:4], gate_ap, ALU.mult, r=['rz', 'gates'], w=['rz'])
        tt('dve', otok, pv[:, :, 0:64], rz[:, 0:4].unsqueeze(2).broadcast_to([128, 4, 64]), ALU.mult, r=[ptrk, 'rz'], w=[otk])

    if 3 in phases:
        with ExitStack() as es:
            KSA = [sbt(es, f"KSA{g}", [128, S_LEN], BF16) for g in range(2)]
            KW = [sbt(es, f"KW{g}", [64, S_LEN], BF16) for g in range(2)]
            KCT = [sbt(es, f"KCT{g}", [64, 256], BF16) for g in range(2)]
            VOZ = [sbt(es, f"VOZ{g}", [128, 2, 129], BF16) for g in range(2)]
            vis = sbt(es, "vis", [128, 2, S_LEN], BF16)
            validm = sbt(es, "validm", [128, 32, 64], F32)
            bonus = sbt(es, "bonus", [128, 32, 64], F32)
            Rm = sbt(es, "Rm3", [128, 128], BF16)
            for g in range(2):
                dma(KSA[g][0:64, :], FM[1792 + g * 64:1792 + (g + 1) * 64, :], r=['FM'], w=[('KSA', g)])
                dma(KSA[g][64:128, :], I['E'], w=[('KSAe', g)])
                dma(KW[g], FM[1920 + g * 64:1920 + (g + 1) * 64, :], r=['FM'], w=[('KW', g)])
                mset('pool', VOZ[g], 0.0, w=[('VOZ', g)])
            dma(Rm, I['R'], w=['Rm'])
            with ExitStack() as ec:
                XC = sbt(ec, "XC", [128, S_LEN], BF16)
                W1 = sbt(ec, "W1", [128, 32, 256], BF16)
                W1s2 = [sbt(ec, f"W1s{i}", [128, 8, 256], F32) for i in range(2)]
                W2s = sbt(ec, "W2s", [128, 2, 64], F32)
                W2 = sbt(ec, "W2", [128, 2, 64], BF16)
                peT = sbt(ec, "peT", [128, 32], F32)
                peTb = sbt(ec, "peTb", [128, 32], BF16)
                b1 = sbt(ec, "b1", [128, 2], F32)
                btot = sbt(ec, "btot", [128, 2], F32)
                b2c = sbt(ec, "b2c", [64, 1], F32)
                b2r = sbt(ec, "b2r", [128, 64], F32)
                hT = [sbt(ec, f"hT{i}", [128, 256], BF16) for i in range(4)]
                xg = sbt(ec, "xg", [128, 256], F32)
                ug = sbt(ec, "ug", [128, 256], F32)
                kx = sbt(ec, "kx", [64, 256], BF16)
                cC = sbt(ec, "cC", [64, 2, 256], F32)
                k1 = sbt(ec, "k1", [64, 256], F32)
                k2 = sbt(ec, "k2", [64, 256], F32)
                ph = [pst(ec, f"ph{i}") for i in range(2)]
                pk2 = pst(ec, "pk2")
                pb1 = pst(ec, "pb1")
                dma(cC[:, 0, :], I['cosC'][0:64, :], w=['cC0'])
                dma(cC[:, 1, :], I['sinC'][0:64, :], w=['cC1'])
                for which in ('k', 'v'):
                    pre = 'cmp' + which
                    dma(XC, FM[1536:1664, :] if which == 'k' else FM[1664:1792, :], r=['FM'], w=['XC'])
                    w1v = I[pre + '_w1'].rearrange("(l d) h -> d l h", d=64)
                    for lq in range(4):
                        W1s = W1s2[lq % 2]
                        for half in range(2):
                            dma(W1s[half * 64:(half + 1) * 64, :, :], w1v[:, lq * 8:(lq + 1) * 8, :], w=[('W1s', lq % 2, half)])
                        cp('pool' if lq % 2 == 0 else 'dve', W1[:, lq * 8:(lq + 1) * 8, :], W1s, r=[('W1s', lq % 2, 0), ('W1s', lq % 2, 1)], w=[('W1', lq)])
                    dma(W2s, I[pre + '_w2'].rearrange("(c p) d -> p c d", p=128), w=['W2s'])
                    cp('pool', W2, W2s, r=['W2s'], w=['W2'])
                    for half in range(2):
                        dma(peT[half * 64:(half + 1) * 64, :], I[pre + '_peT'], w=[('peT', half)])
                    cp('pool', peTb, peT, r=[('peT', 0), ('peT', 1)], w=['peTb'])
                    dma(b1, I[pre + '_b1'], w=['b1'])
                    if which == 'k':
                        dma(b2c, I['cmpk_b2'], w=['b2c'])
                    else:
                        dma(b2r, I['cmpv_b2'].partition_broadcast(128), w=['b2r'])
                    for hc in range(2):
                        for l in range(32):
                            mm(pb1[:, hc:hc + 1], W1[0:64, l, hc * 128:(hc + 1) * 128], peTb[0:64, l:l + 1], start=(l == 0), stop=(l == 31),
                               r=[('W1', 0), ('W1', 1), ('W1', 2), ('W1', 3), 'peTb'], w=['pb1'])
                    tt('dve', btot, pb1[:, 0:2], b1, ALU.add, r=['pb1', 'b1'], w=['btot'])
                    xcv = XC.rearrange("p (c s) -> p c s", s=16)
                    for g in range(2):
                        for hc in range(2):
                            pp = ph[hc]
                            for l in range(32):
                                rhs = xcv[g * 64:(g + 1) * 64, 0:255, l] if l < 16 else xcv[g * 64:(g + 1) * 64, 1:256, l - 16]
                                mm(pp[:, 0:255], W1[g * 64:(g + 1) * 64, l, hc * 128:(hc + 1) * 128], rhs, start=(l == 0), stop=(l == 31),
                                   r=[('W1', 0), ('W1', 1), ('W1', 2), ('W1', 3), 'XC'], w=[('ph', hc)])
                            hh = hT[g * 2 + hc]
                            hk = ('hT', g * 2 + hc)
                            ts('dve', xg[:, 0:255], pp[:, 0:255], btot[:, hc:hc + 1], ALU.add, r=[('ph', hc), 'btot'], w=['xg'])
                            tt('dve', ug[:, 0:255], xg[:, 0:255], xg[:, 0:255], ALU.mult, r=['xg'], w=['ug'])
                            ts('dve', ug[:, 0:255], ug[:, 0:255], 0.044715, ALU.mult, 1.0, ALU.add, r=['ug'], w=['ug'])
                            tt('dve', ug[:, 0:255], ug[:, 0:255], xg[:, 0:255], ALU.mult, r=['ug', 'xg'], w=['ug'])
                            act(ug[:, 0:255], ug[:, 0:255], AF.Exp, scale=-1.59576912, r=['ug'], w=['ug'])
                            ts('dve', ug[:, 0:255], ug[:, 0:255], 1.0, ALU.add, r=['ug'], w=['ug'])
                            recip(ug[:, 0:255], ug[:, 0:255], r=['ug'], w=['ug'])
                            tt('dve', hh[:, 0:255], xg[:, 0:255], ug[:, 0:255], ALU.mult, r=['ug', 'xg'], w=[hk])
                        if which == 'k':
                            for hc in range(2):
                                mm(pk2[0:64, 0:255], W2[:, hc, :], hT[g * 2 + hc][:, 0:255], start=(hc == 0), stop=(hc == 1),
                                   r=['W2', ('hT', g * 2 + hc)], w=['pk2'])
                            ts('dve', k1[:, 0:255], pk2[0:64, 0:255], b2c[:, 0:1], ALU.add, r=['pk2', 'b2c'], w=['k1'])
                            cp('dve', kx[:, 0:255], k1[:, 0:255], r=['k1'], w=['kx'])
                            mm(pk2[0:64, 256:511], Rm[0:64, 0:64], kx[:, 0:255], r=['Rm', 'kx'], w=['pk2'])
                            tt('dve', k2[:, 0:255], pk2[0:64, 256:511], cC[:, 1, 0:255], ALU.mult, r=['pk2', 'cC1'], w=['k2'])
                            tt('dve', k1[:, 0:255], k1[:, 0:255], cC[:, 0, 0:255], ALU.mult, r=['k1', 'cC0'], w=['k1'])
                            tt('dve', KCT[g][:, 0:255], k1[:, 0:255], k2[:, 0:255], ALU.add, r=['k1', 'k2'], w=[('KCT', g)])
                        else:
                            for ct in range(2):
                                M = 128 if ct == 0 else 127
                                for hc in range(2):
                                    mm(pk2[0:M, ct * 64:(ct + 1) * 64], hT[g * 2 + hc][:, ct * 128:ct * 128 + M], W2[:, hc, :],
                                       start=(hc == 0), stop=(hc == 1), r=['W2', ('hT', g * 2 + hc)], w=['pk2'])
                                tt('dve', VOZ[g][0:M, ct, 0:64], pk2[0:M, ct * 64:(ct + 1) * 64], b2r[0:M, :], ALU.add,
                                   r=['pk2', 'b2r'], w=[('VOZ', g)])
                dma(vis, I['vis'], w=['vis'])
                dma(validm, I['validm'], w=['validm'])
                dma(bonus, I['bonus'], w=['bonus'])
                for g in range(2):
                    dma(VOZ[g][:, :, 64:128], I['ovl'], w=[('VOZo', g)])
                    mset('pool', VOZ[g][:, :, 128:129], 1.0, w=[('VOZ1', g)])
            S.barrier()
            QA = [sbt(es, f"QA{i}", [128, 4, 128], BF16) for i in range(2)]
            pcm = [sbt(es, f"pcm{i}", [128, 512], BF16) for i in range(2)]
            pt = [sbt(es, f"ptn{i}", [128, 512], BF16) for i in range(3)]
            osb = sbt(es, "osb3", [65, 512], F32)
            rz = sbt(es, "rz3", [128, 4], F32)
            rzA = sbt(es, "rzA", [128, 8], F32)
            ocmp = [sbt(es, f"ocmp{i}", [128, 4, 64], F32) for i in range(3)]
            oslc = sbt(es, "oslc", [128, 4, 64], F32)
            owin = [sbt(es, f"owin{i}", [128, 4, 64], F32) for i in range(2)]
            pendB = []
            pendN = []
            onsa = [sbt(es, f"onsa{i}", [128, 4, 64], F32) for i in range(2)]
            impw = sbt(es, "impw", [128, 4, 64], F32)
            score_t = sbt(es, "score", [128, 64], F32)
            tmpm = sbt(es, "tmpm", [128, 64], F32)
            m8 = sbt(es, "m8", [128, 16], F32)
            negm = sbt(es, "negm", [128, 128], BF16)
            pS = [pst(es, f"pS3{i}") for i in range(3)]
            pO = [pst(es, f"pO3{i}") for i in range(3)]
            pcz = [pst(es, f"pcz{i}") for i in range(2)]
            mset('pool', negm, 0.0, w=['negm'])
            vk = [('VOZ', 0), ('VOZ', 1), ('VOZo', 0), ('VOZo', 1), ('VOZ1', 0), ('VOZ1', 1)]
            items = [(g, qt) for g in range(2) for qt in range(LIM.get('p3q', 32))]

            def ctx(i):
                g, qt = items[i]
                s2 = i % 2
                qa = QA[s2]
                return g, qt, s2, qa, qa.rearrange("p h q -> p (h q)"), gates[:, qt, g * 12:(g + 1) * 12].rearrange("p (h b) -> p h b", b=3)

            def stageA1a(i):
                g, qt, s2, qa, qa2, gsl = ctx(i)
                dma(qa[0:64, :, :], FM[1024 + g * 256:1024 + (g + 1) * 256, qt * 128:(qt + 1) * 128].rearrange("(h d) q -> d h q", h=4),
                    r=['FM'], w=[('qa', s2, 'q')])
                nct = 1 if qt < 16 else 2
                for ct in range(nct):
                    M = 128 if ct == 0 else 127
                    mm(pcz[ct][0:M, :], KCT[g][:, ct * 128:ct * 128 + M], qa2[0:64, :], r=[('KCT', g), ('qa', s2, 'q')], w=[('pcz', ct)])
                    act(pcm[ct][0:M, :], pcz[ct][0:M, :], AF.Exp, scale=0.125, r=[('pcz', ct)], w=[('pcm', ct)])
                    tt('pool', pcm[ct][0:M, :].rearrange("p (h q) -> p h q", h=4), pcm[ct][0:M, :].rearrange("p (h q) -> p h q", h=4),
                       vis[0:M, ct, qt * 128:(qt + 1) * 128].unsqueeze(1).broadcast_to([M, 4, 128]), ALU.mult,
                       r=[('pcm', ct), 'vis'], w=[('pcm', ct)])

            def stageA1b(i):
                g, qt, s2, qa, qa2, gsl = ctx(i)
                nct = 1 if qt < 16 else 2
                oc = ocmp[i % 3]
                for h in range(4):
                    pz = pcz[h // 2]
                    for ct in range(nct):
                        M = 128 if ct == 0 else 127
                        mm(pz[:, (h % 2) * 256:(h % 2) * 256 + 129], pcm[ct][0:M, h * 128:(h + 1) * 128], VOZ[g][0:M, ct, :],
                           start=(ct == 0), stop=(ct == nct - 1), r=[('pcm', 0), ('pcm', 1)] + vk, w=[('pcz', h // 2)])
                for hb in range(2):
                    pzv = pcz[hb].rearrange("p (h c) -> p h c", h=2)
                    ts('dve', rzA[:, 2 * hb:2 * hb + 2], pzv[:, :, 128], 1e-30, ALU.max, r=[('pcz', hb)], w=['rzA'])
                recip(rzA[:, 0:4], rzA[:, 0:4], r=['rzA'], w=['rzA'])
                for hb in range(2):
                    pzv = pcz[hb].rearrange("p (h c) -> p h c", h=2)
                    tt('dve', impw[:, 2 * hb:2 * hb + 2, :], pzv[:, :, 64:128], rzA[:, 2 * hb:2 * hb + 2].unsqueeze(2).broadcast_to([128, 2, 64]),
                       ALU.mult, r=[('pcz', hb), 'rzA'], w=['impw'])
                tt('dve', rzA[:, 4:8], rzA[:, 0:4], gsl[:, :, 0], ALU.mult, r=['rzA', 'gates'], w=['rzA2'])
                for hb in range(2):
                    pzv = pcz[hb].rearrange("p (h c) -> p h c", h=2)
                    tt('dve', oc[:, 2 * hb:2 * hb + 2, :], pzv[:, :, 0:64], rzA[:, 4 + 2 * hb:6 + 2 * hb].unsqueeze(2).broadcast_to([128, 2, 64]),
                       ALU.mult, r=[('pcz', hb), 'rzA2'], w=[('ocmp', i % 3)])
                red('dve', score_t, impw.rearrange("p h j -> p j h"), ALU.add, r=['impw'], w=['score'])
                tt('dve', score_t, score_t, validm[:, qt, :], ALU.mult, r=['score', 'validm'], w=['score'])
                tt('dve', score_t, score_t, bonus[:, qt, :], ALU.add, r=['score', 'bonus'], w=['score'])
                S.op('dve', lambda e: e.max(out=m8[:, 0:8], in_=score_t), reads=['score'], writes=['m8'])
                S.op('dve', lambda e: e.match_replace(out=tmpm, in_to_replace=m8[:, 0:8], in_values=score_t, imm_value=-2.0),
                     reads=['score', 'm8'], writes=['tmpm'])
                S.op('dve', lambda e: e.max(out=m8[:, 8:16], in_=tmpm), reads=['tmpm'], writes=['m8b'])
                ts('dve', negm[:, 64:128], score_t, m8[:, 15:16], ALU.is_lt, NEGM, ALU.mult, r=['score', 'm8b'], w=['negm'])

            def stageA2(i):
                g, qt, s2, qa, qa2, gsl = ctx(i)
                pT = pcz[0].bitcast(BF16)
                tp(pT[:, 0:128], negm, identb, r=['negm', 'identb'], w=[('pcz', 0)])
                cp('dve', qa[64:128, :, :], pT[64:128, 0:128].unsqueeze(1).broadcast_to([64, 4, 128]), r=[('pcz', 0)], w=[('qa', s2, 'm')])

            def stageB(i):
                g, qt, s2, qa, qa2, gsl = ctx(i)
                qq = [('qa', s2, 'q')]
                qm = [('qa', s2, 'q'), ('qa', s2, 'm')]
                tiles = []
                w0 = max(0, qt - 4)
                for kt in range(w0, qt + 1):
                    tiles.append(('w', kt, kt == w0, kt == qt))
                for kt in range(qt + 1):
                    tiles.append(('s', kt, kt == 0, kt == qt))

                def score(n):
                    br, kt, first, last = tiles[n]
                    if br == 's':
                        mm(pS[n % 3], KSA[g][:, kt * 128:(kt + 1) * 128], qa2, r=[('KSA', g), ('KSAe', g)] + qm, w=[('pS', n % 3)])
                    else:
                        mm(pS[n % 3], KW[g][:, kt * 128:(kt + 1) * 128], qa2[0:64, :], r=[('KW', g)] + qq, w=[('pS', n % 3)])
                score(0)
                if len(tiles) > 1:
                    score(1)
                for n in range(len(tiles)):
                    br, kt, first, last = tiles[n]
                    if n + 2 < len(tiles):
                        score(n + 2)
                    if n == 1 and i + 1 < len(items):
                        stageA1b(i + 1)
                    if n == min(3, len(tiles) - 1) and pendN:
                        pendN.pop(0)()
                    b3 = n % 3
                    act(pt[b3], pS[b3], AF.Exp, scale=0.125, r=[('pS', b3)], w=[('pt', b3)])
                    p3 = pt[b3].rearrange("p (h q) -> p h q", h=4)
                    if kt == qt:
                        tt('pool', p3, p3, tri.unsqueeze(1).broadcast_to([128, 4, 128]), ALU.mult, r=[('pt', b3), 'tri'], w=[('pt', b3)])
                    if br == 'w' and kt == qt - 4:
                        tt('pool', p3, p3, atri.unsqueeze(1).broadcast_to([128, 4, 128]), ALU.mult, r=[('pt', b3), 'atri'], w=[('pt', b3)])
                    bi = (i % 2) if br == 's' else 2
                    V = VS if br == 's' else VW
                    mm(pO[bi][0:65, :], V[:, kt, g, :], pt[b3], start=first, stop=last, r=['VS', 'VW', ('pt', b3)], w=[('pO', bi)])
                    if last and br == 'w':
                        def fin_w(bi=bi, gsl=gsl, s2=s2):
                            finalize(pO[bi], osb, [('pO', bi), 'osb'], pO[bi], ('pO', bi), rz, owin[s2], ('owin', s2), gate_ap=gsl[:, :, 2])
                        pendB.append((n + 3, fin_w))
                    if last and br == 's':
                        def fin_s(bi=bi, gsl=gsl, s2=s2, g=g, qt=qt, i=i):
                            finalize(pO[bi], osb, [('pO', bi), 'osb'], pO[bi], ('pO', bi), rz, oslc, 'oslc', gate_ap=gsl[:, :, 1])
                            o2 = i % 2
                            tt('dve', onsa[o2], ocmp[i % 3], oslc, ALU.add, r=[('ocmp', i % 3), 'oslc'], w=[('onsa', o2)])
                            tt('dve', onsa[o2], onsa[o2], owin[s2], ALU.add, r=[('onsa', o2), ('owin', s2)], w=[('onsa', o2)])
                            dma(OS[qt * 128:(qt + 1) * 128, 512 + g * 256:512 + (g + 1) * 256], onsa[o2].rearrange("p h d -> p (h d)"),
                                r=[('onsa', o2)], w=[('OSn', g, qt)], q='sp')
                        pendN.append(fin_s)
                    while pendB and (pendB[0][0] <= n or n == len(tiles) - 1):
                        pendB.pop(0)[1]()

            stageA1a(0)
            stageA1b(0)
            stageA2(0)
            for i in range(len(items)):
                if i + 1 < len(items):
                    stageA1a(i + 1)
                stageB(i)
                if i + 1 < len(items):
                    stageA2(i + 1)
            while pendN:
                pendN.pop(0)()
        S.barrier()

    if 1 in phases or 2 in phases or 3 in phases:
        esV.close()
    esW = ExitStack()
    PREF = [False]
    if 2 in phases:
        with ExitStack() as es:
            QFA = [sbt(es, f"QFA{i}", [128, S_LEN], BF16) for i in range(2)]
            KFA = [sbt(es, f"KFA{i}", [128, S_LEN], BF16) for i in range(2)]
            pt = [sbt(es, f"pt{i}", [128, 512], BF16) for i in range(4)]
            osb = sbt(es, "osb", [65, 512], F32)
            rz = sbt(es, "rz", [128, 4], F32)
            otok = [sbt(es, f"otok{i}", [128, 4, 64], F32) for i in range(2)]
            pS = [pst(es, f"pS{i}") for i in range(4)]
            pO = [pst(es, f"pO{i}") for i in range(2)]
            ptr = pst(es, "ptr")
            if 4 in phases:
                Wu = nc.alloc_sbuf_tensor_at("sb_Wu", [128, 8 * 4096], BF16, offset=229376 - 64 - 65536).ap().rearrange("p (k n) -> p k n", k=8)
                Wo = nc.alloc_sbuf_tensor_at("sb_Wo", [128, 8 * D], BF16, offset=229376 - 64 - 65536 - 16384).ap().rearrange("p (k n) -> p k n", k=8)
                wstf = [sbt(es, f"wstf{i}", [128, 2048], F32) for i in range(2)]
                PREF[0] = True
                assert nc.sbuf_bytes_remaining >= 81984 + 256, nc.sbuf_bytes_remaining
            for i in range(2):
                mset('pool', QFA[i], 0.0, w=[('qfa', i, 'q'), ('qfa', i, 'h'), ('qfa', i, 'l')])
                mset('pool', KFA[i], 0.0, w=[('kfa', i)])
                mset('pool', KFA[i][64:65, :], 1.0, w=[('kfa', i)])
                mset('pool', KFA[i][96:97, :], 1.0, w=[('kfa', i)])
            oi_box = [0]
            pend = []
            for h in range(LIM.get('p2h', 8)):
                s2 = h % 2
                dma(QFA[s2][0:64, :], FM[h * 64:(h + 1) * 64, :], r=['FM'], w=[('qfa', s2, 'q')])
                dma(QFA[s2][64:65, :], chi[h:h + 1, :], r=['chi'], w=[('qfa', s2, 'h')])
                dma(QFA[s2][96:97, :], clo[h:h + 1, :], r=['clo'], w=[('qfa', s2, 'l')])
                dma(KFA[s2][0:64, :], FM[512 + h * 64:512 + (h + 1) * 64, :], r=['FM'], w=[('kfa', s2)])
                qk = [('qfa', s2, 'q'), ('qfa', s2, 'h'), ('qfa', s2, 'l')]
                if PREF[0]:
                    dma(wstf[0][:, 0:D], I['w_out'][h * 128:(h + 1) * 128, :], w=[('wstf', 0)])
                    cp('pool', Wo[:, h, :], wstf[0][:, 0:D], r=[('wstf', 0)], w=['Wo'])
                    for hf in range(2):
                        dma(wstf[1 - hf], I['w_up'][h * 128:(h + 1) * 128, hf * 2048:(hf + 1) * 2048], w=[('wstf', 1 - hf)])
                        cp('pool', Wu[:, h, hf * 2048:(hf + 1) * 2048], wstf[1 - hf], r=[('wstf', 1 - hf)], w=['Wu'])
                tiles = []
                for qi in range(LIM.get('p2q', 8)):
                    nkt = 4 * qi + 4
                    for kt in range(nkt):
                        j = kt - 4 * qi
                        tiles.append((qi, kt, 128 * j if j > 0 else 0, j >= 0, kt == 0, kt == nkt - 1))

                def score(n):
                    qi, kt, c0, dg, first, last = tiles[n]
                    mm(pS[n % 4][:, c0:512], KFA[s2][:, kt * 128:(kt + 1) * 128], QFA[s2][:, qi * 512 + c0:(qi + 1) * 512],
                       r=qk + [('kfa', s2)], w=[('pS', n % 4)])
                score(0)
                score(1)
                score(2)
                for n in range(len(tiles)):
                    qi, kt, c0, dg, first, last = tiles[n]
                    if n + 3 < len(tiles):
                        score(n + 3)
                    b3 = n % 4
                    act(pt[b3][:, c0:512], pS[b3][:, c0:512], AF.Exp, bias=negc[:, kt, h:h + 1], scale=0.125,
                        r=[('pS', b3), 'negc'], w=[('pt', b3)])
                    if dg:
                        tt('pool', pt[b3][:, c0:c0 + 128], pt[b3][:, c0:c0 + 128], tri, ALU.mult, r=[('pt', b3), 'tri'], w=[('pt', b3)])
                    po = pO[qi % 2]
                    mm(po[0:65, c0:512], VF[:, kt, h, :], pt[b3][:, c0:512], start=first, stop=last,
                       r=['VF', ('pt', b3)], w=[('pO', qi % 2)])
                    if last:
                        def fin_fox(po=po, qi=qi, h=h):
                            o2 = oi_box[0] % 2
                            oi_box[0] += 1
                            finalize(po, osb, [('pO', qi % 2), 'osb'], ptr, 'ptr', rz, otok[o2], ('otok', o2), ev='dve')
                            dma(OS[qi * 512:(qi + 1) * 512, h * 64:(h + 1) * 64].rearrange("(j p) d -> p j d", p=128), otok[o2],
                                r=[('otok', o2)], w=[('OS', h, qi)], q='sp')
                        pend.append((n + 3, fin_fox))
                    while pend and (pend[0][0] <= n or n == len(tiles) - 1):
                        pend.pop(0)[1]()
        S.barrier()

    if 1 in phases or 2 in phases or 3 in phases:
        es12.close()
        S.barrier()

    if 4 in phases:
        with ExitStack() as es:
            if not PREF[0]:
                Wo = sbt(es, "Wo", [128, 8, D], BF16)
                Wu = sbt(es, "Wu", [128, 8, 4096], BF16)
            Wd = sbt(es, "Wd", [128, 32, D], BF16)
            gfn = sbt(es, "gfn", [128, D], F32)
            gml = sbt(es, "gml", [128, D], F32)
            gfi = sbt(es, "gfi", [128, D], F32)
            with ExitStack() as ew:
                wst = [sbt(ew, f"wst4{i}", [128, 2048], F32) for i in range(2)]
                assert (not PREF[0]) or nc.sbuf_bytes_remaining >= 81984 + 256, nc.sbuf_bytes_remaining
                wi = 0
                for kc in range(0 if PREF[0] else 8):
                    st = wst[wi % 2]
                    dma(st[:, 0:D], I['w_out'][kc * 128:(kc + 1) * 128, :], w=[('wst', wi % 2)])
                    cp('pool', Wo[:, kc, :], st[:, 0:D], r=[('wst', wi % 2)], w=['Wo'])
                    wi += 1
                for kc in range(0 if PREF[0] else 8):
                    for hf in range(2):
                        st = wst[wi % 2]
                        dma(st, I['w_up'][kc * 128:(kc + 1) * 128, hf * 2048:(hf + 1) * 2048], w=[('wst', wi % 2)])
                        cp('pool' if hf == 0 else 'dve', Wu[:, kc, hf * 2048:(hf + 1) * 2048], st, r=[('wst', wi % 2)], w=['Wu'])
                        wi += 1
                for hc in range(0, 32, 2):
                    st = wst[wi % 2]
                    dma(st.rearrange("p (c d) -> p c d", c=2), I['w_down'][hc * 128:(hc + 2) * 128, :].rearrange("(c p) d -> p c d", p=128),
                        w=[('wst', wi % 2)])
                    cp('pool' if (hc // 2) % 2 == 0 else 'dve', Wd[:, hc:hc + 2, :], st.rearrange("p (c d) -> p c d", c=2), r=[('wst', wi % 2)], w=['Wd'])
                    wi += 1
                dma(gfn[:, 0:512], I['g_fox'].partition_broadcast(128), w=['gfn0'])
                dma(gfn[:, 512:1024], I['g_nsa'].partition_broadcast(128), w=['gfn1'])
                dma(gml, I['g_mlp'].partition_broadcast(128), w=['gml'])
                dma(gfi, I['g_final'].partition_broadcast(128), w=['gfi'])
            S.barrier()
            xh = [sbt(es, f"xh{i}", [128, D], F32) for i in range(2)]
            ot = [sbt(es, f"ot{i}", [128, D], F32) for i in range(2)]
            sq = sbt(es, "sq4", [128, D], F32)
            ss = sbt(es, "ss4", [128, 2], F32)
            rstd = sbt(es, "rstd4", [128, 2], F32)
            ssm = sbt(es, "ssm4", [128, 2], F32)
            rstdm = sbt(es, "rstdm4", [128, 2], F32)
            yb = sbt(es, "yb", [128, D], BF16)
            ya = sbt(es, "ya", [128, D], BF16)
            yT = sbt(es, "yT", [128, 8, 128], BF16)
            h1T = [sbt(es, f"h1T{i}", [128, 8, 128], BF16) for i in range(2)]
            rl = [sbt(es, f"rl{i}", [128, 128], F32) for i in range(2)]
            hid = sbt(es, "hid", [128, 32, 128], BF16)
            fin = [sbt(es, f"fin{i}", [128, D], F32) for i in range(2)]
            psT = pst(es, "psT4", BF16)
            pP = [pst(es, f"pP{i}") for i in range(2)]
            pA = [pst(es, f"pA{i}") for i in range(2)]
            pU = [pst(es, f"pU{i}") for i in range(3)]
            assert (not PREF[0]) or nc.sbuf_bytes_remaining >= 81984 + 256, nc.sbuf_bytes_remaining
            LIM['_rem4'] = nc.sbuf_bytes_remaining

            def p4load_o(kt):
                dma(ot[kt % 2], OS[kt * 128:(kt + 1) * 128, :], r=['OS'], w=[('ot', kt % 2)])

            def p4load_x(kt):
                dma(xh[kt % 2], I['x'][kt * 128:(kt + 1) * 128, :], w=[('xh', kt % 2)])

            def P_a(kt):
                s2 = kt % 2
                for gi in range(2):
                    tt('dve', sq[:, gi * 512:(gi + 1) * 512], ot[s2][:, gi * 512:(gi + 1) * 512], ot[s2][:, gi * 512:(gi + 1) * 512], ALU.mult,
                       r=[('ot', s2)], w=['sq'])
                red('dve', ss, sq.rearrange("p (g d) -> p g d", g=2), ALU.add, r=['sq'], w=['ss'])
                act(rstd, ss, AF.Ln, bias=epsb[:, 0:1], scale=1.0 / 512, r=['ss', 'epsb'], w=['rstd'])
                act(rstd, rstd, AF.Exp, scale=-0.5, r=['rstd'], w=['rstd'])
                for gi in range(2):
                    stt('dve', ya[:, gi * 512:(gi + 1) * 512], ot[s2][:, gi * 512:(gi + 1) * 512], rstd[:, gi:gi + 1], gfn[:, gi * 512:(gi + 1) * 512],
                        ALU.mult, ALU.mult, r=[('ot', s2), 'rstd', 'gfn0', 'gfn1'], w=['ya'])
                if kt + 2 < NT:
                    p4load_o(kt + 2)

            def P_a2(kt):
                for kc in range(8):
                    tp(psT[:, kc * 128:(kc + 1) * 128], ya[:, kc * 128:(kc + 1) * 128], identb, r=['ya', 'identb'], w=['psT'])
                cp('act', yT, psT.rearrange("p (k t) -> p k t", k=8), r=['psT'], w=['yT'])

            def P_b(kt):
                s2 = kt % 2
                for hf in range(2):
                    for kc in range(8):
                        mm(pP[hf], yT[:, kc, :], Wo[:, kc, hf * 512:(hf + 1) * 512], start=(kc == 0), stop=(kc == 7), r=['yT', 'Wo'], w=[('pP', hf)])
                    tt('dve', xh[s2][:, hf * 512:(hf + 1) * 512], pP[hf], xh[s2][:, hf * 512:(hf + 1) * 512], ALU.add,
                       r=[('pP', hf), ('xh', s2)], w=[('xh', s2)])
                tt('dve', sq, xh[s2], xh[s2], ALU.mult, r=[('xh', s2)], w=['sq'])
                red('dve', ss[:, 0:1], sq, ALU.add, r=['sq'], w=['ss'])
                act(rstd[:, 0:1], ss[:, 0:1], AF.Ln, bias=epsb[:, 0:1], scale=1.0 / D, r=['ss', 'epsb'], w=['rstd'])
                act(rstd[:, 0:1], rstd[:, 0:1], AF.Exp, scale=-0.5, r=['rstd'], w=['rstd'])
                stt('dve', yb, xh[s2], rstd[:, 0:1], gml, ALU.mult, ALU.mult, r=[('xh', s2), 'rstd', 'gml'], w=['yb'])

            def P_c(kt):
                s2 = kt % 2
                for kc in range(8):
                    tp(psT[:, kc * 128:(kc + 1) * 128], yb[:, kc * 128:(kc + 1) * 128], identb, r=['yb', 'identb'], w=['psT'])
                cp('act', h1T[s2], psT.rearrange("p (k t) -> p k t", k=8), r=['psT'], w=[('h1T', s2)])

            p4load_o(0)
            p4load_x(0)
            if NT > 1:
                p4load_o(1)
                p4load_x(1)
            P_a(0)
            P_a2(0)
            if NT > 1:
                P_a(1)
            P_b(0)
            P_c(0)
            for kt in range(NT):
                s2 = kt % 2
                nxt = kt + 1 < NT
                for hc in range(32):
                    pu = pU[hc % 3]
                    for kc in range(8):
                        mm(pu[:, 0:128], Wu[:, kc, hc * 128:(hc + 1) * 128], h1T[s2][:, kc, :], start=(kc == 0), stop=(kc == 7),
                           r=['Wu', ('h1T', s2)], w=[('pU', hc % 3)])
                    act(rl[hc % 2], pu[:, 0:128], AF.Relu, r=[('pU', hc % 3)], w=[('rl', hc % 2)])
                    tt('pool', hid[:, hc, :], rl[hc % 2], rl[hc % 2], ALU.mult, r=[('rl', hc % 2)], w=['hid'])
                    if nxt and hc == 3:
                        P_a2(kt + 1)
                    if nxt and hc == 6:
                        P_b(kt + 1)
                    if kt + 2 < NT and hc == 16:
                        P_a(kt + 2)
                    if nxt and hc == 26:
                        P_c(kt + 1)
                for hf in range(2):
                    for hc in range(32):
                        mm(pA[hf], hid[:, hc, :], Wd[:, hc, hf * 512:(hf + 1) * 512], start=(hc == 0), stop=(hc == 31), r=['hid', 'Wd'], w=[('pA', hf)])
                    tt('dve', xh[s2][:, hf * 512:(hf + 1) * 512], pA[hf], xh[s2][:, hf * 512:(hf + 1) * 512], ALU.add,
                       r=[('pA', hf), ('xh', s2)], w=[('xh', s2)])
                tt('dve', sq, xh[s2], xh[s2], ALU.mult, r=[('xh', s2)], w=['sq'])
                red('dve', ssm[:, 0:1], sq, ALU.add, r=['sq'], w=['ssm'])
                act(rstdm[:, 0:1], ssm[:, 0:1], AF.Ln, bias=epsb[:, 0:1], scale=1.0 / D, r=['ssm', 'epsb'], w=['rstdm'])
                act(rstdm[:, 0:1], rstdm[:, 0:1], AF.Exp, scale=-0.5, r=['rstdm'], w=['rstdm'])
                stt('dve', fin[s2], xh[s2], rstdm[:, 0:1], gfi, ALU.mult, ALU.mult, r=[('xh', s2), 'rstdm', 'gfi'], w=[('fin', s2)])
                dma(out_d[kt * 128:(kt + 1) * 128, :], fin[s2], r=[('fin', s2)], w=[('out', kt)])
                if kt + 2 < NT:
                    p4load_x(kt + 2)
    esW.close()
    S.finish()
    LIM['_cnt'] = dict(S.cnt)
    LIM['_dma'] = {q: list(v) for q, v in S.dma_use.items()}
    S.emit()
    es_all.close()
    return nc


_CONSTS = None


def make_in_maps(inputs):
    global _CONSTS
    if _CONSTS is None:
        _CONSTS = _consts()
    c = _CONSTS
    f = lambda a: np.ascontiguousarray(np.asarray(a, dtype=np.float32))
    shared = {
        'w_in': f(inputs['w_in'][0]), 'w_out': f(inputs['w_out'][0]), 'w_up': f(inputs['w_up'][0]), 'w_down': f(inputs['w_down'][0]),
        'g_attn': f(inputs['g_attn']).reshape(1, D), 'g_mlp': f(inputs['g_mlp']).reshape(1, D), 'g_final': f(inputs['g_final']).reshape(1, D),
        'g_fox': f(inputs['g_fox']).reshape(1, 512), 'g_nsa': f(inputs['g_nsa']).reshape(1, 512),
        'b_f': f(inputs['b_f']).reshape(1, 8), 'b_gate': f(inputs['b_gate']).reshape(1, 24),
        'cmpk_peT': f(np.asarray(inputs['cmpk_pe'][0]).T), 'cmpk_w1': f(inputs['cmpk_w1'][0]),
        'cmpk_b1': f(np.asarray(inputs['cmpk_b1'][0]).reshape(2, 128).T), 'cmpk_w2': f(inputs['cmpk_w2'][0]),
        'cmpk_b2': f(inputs['cmpk_b2'][0]).reshape(64, 1),
        'cmpv_peT': f(np.asarray(inputs['cmpv_pe'][0]).T), 'cmpv_w1': f(inputs['cmpv_w1'][0]),
        'cmpv_b1': f(np.asarray(inputs['cmpv_b1'][0]).reshape(2, 128).T), 'cmpv_w2': f(inputs['cmpv_w2'][0]),
        'cmpv_b2': f(inputs['cmpv_b2'][0]).reshape(1, 64),
    }
    shared.update(c)
    x = np.asarray(inputs['x'], dtype=np.float32)
    return [dict(shared, x=np.ascontiguousarray(x[b])) for b in range(x.shape[0])]


def kernel(**inputs):
    nc = build()
    in_maps = make_in_maps(inputs)
    res = run_bass_kernel_spmd(nc, in_maps, core_ids=list(range(8)))
    return np.stack([np.asarray(r['out'], dtype=np.float32) for r in res.results], axis=0)
```

```python
from contextlib import ExitStack
import numpy as np
import ml_dtypes
import concourse.bass as bass
import concourse.mybir as mybir
from concourse.bass_utils import run_bass_kernel_spmd

F32 = mybir.dt.float32
BF16 = mybir.dt.bfloat16
AF = mybir.ActivationFunctionType
ALU = mybir.AluOpType
AX = mybir.AxisListType
bf = ml_dtypes.bfloat16

S_LEN = 4096
D = 1024
NT = 32
EPS = 1e-6
NEGM = -30000.0
LIM = {}


class Sched:
    ENG = ('pe', 'act', 'dve', 'pool', 'sp')
    DMAQ = ('sp', 'act', 'pool')

    def __init__(self, nc, ndma=8, selfsync=True):
        self.nc = nc
        self.selfsync = selfsync
        self.semh = {}
        for e in self.ENG:
            self.semh['s_' + e] = nc.alloc_semaphore(name='s_' + e)
        self.ndma = {q: (ndma if q == 'sp' else 8) for q in self.DMAQ}
        for q in self.DMAQ:
            for i in range(self.ndma[q]):
                self.semh[f'd_{q}{i}'] = nc.alloc_semaphore(name=f'd_{q}{i}')
        self.cnt = {e: 0 for e in self.ENG}
        self.prog = {e: [] for e in self.ENG}
        self.seen = {e: {} for e in self.ENG}
        self.bufs = {}
        self.dma_use = {q: [0] * self.ndma[q] for q in self.DMAQ}
        self.dma_rr = {q: 0 for q in self.DMAQ}

    def _deps(self, eng, reads, writes):
        toks = []
        for b in reads:
            st = self.bufs.get(b)
            if st and st[0]:
                toks.append(st[0])
        for b in writes:
            st = self.bufs.get(b)
            if st:
                if st[0]:
                    toks.append(st[0])
                toks.extend(st[1])
        best = {}
        for (sk, val, teng) in toks:
            if teng == eng and (eng == 'pe' or not self.selfsync):
                continue
            if self.seen[eng].get(sk, 0) >= val:
                continue
            if best.get(sk, 0) < val:
                best[sk] = val
        for sk, val in best.items():
            self.seen[eng][sk] = val
        return list(best.items())

    def _mark(self, tok, reads, writes):
        for b in reads:
            self.bufs.setdefault(b, [None, []])[1].append(tok)
        for b in writes:
            self.bufs[b] = [tok, []]

    PSK = {'psT', 'pfm', 'prr', 'ptA', 'ptB', 'pcc', 'pS', 'pO', 'ptr', 'ph', 'pk2', 'pb1', 'pcz', 'pA', 'pU', 'pP'}

    def op(self, eng, fn, reads=(), writes=()):
        ex = [b for b in reads if (b if isinstance(b, str) else b[0]) in self.PSK]
        if ex:
            reads = [b for b in reads if b not in ex]
            writes = list(writes) + ex
        waits = self._deps(eng, reads, writes)
        self.cnt[eng] += 1
        tok = ('s_' + eng, self.cnt[eng], eng)
        self.prog[eng].append(('op', waits, fn))
        self._mark(tok, reads, writes)

    def dma(self, q, out, in_, reads=(), writes=()):
        i = self.dma_rr[q]
        self.dma_rr[q] = (i + 1) % self.ndma[q]
        sk = f'd_{q}{i}'
        prev = self.dma_use[q][i]
        waits = self._deps(q, reads, writes)
        if prev > 0 and self.seen[q].get(sk, 0) < 16 * prev:
            self.seen[q][sk] = 16 * prev
            waits = [w for w in waits if w[0] != sk] + [(sk, 16 * prev)]
        self.dma_use[q][i] = prev + 1
        tok = (sk, 16 * (prev + 1), 'dma')
        self.prog[q].append(('dma', waits, (out, in_, sk)))
        self._mark(tok, reads, writes)

    def _all_waits(self, eng, own=False):
        waits = []
        for e in self.ENG:
            if (e != eng or own) and self.cnt[e] > 0 and self.seen[eng].get('s_' + e, 0) < self.cnt[e]:
                self.seen[eng]['s_' + e] = self.cnt[e]
                waits.append(('s_' + e, self.cnt[e]))
        for q in self.DMAQ:
            for i in range(self.ndma[q]):
                v = 16 * self.dma_use[q][i]
                sk = f'd_{q}{i}'
                if v > 0 and self.seen[eng].get(sk, 0) < v:
                    self.seen[eng][sk] = v
                    waits.append((sk, v))
        return waits

    def barrier(self):
        cnt0 = dict(self.cnt)
        for e in self.ENG:
            w = self._all_waits(e, own=True)
            if w:
                self.prog[e].append(('wait', w, None))
        self.bufs = {}

    def finish(self):
        self.prog['sp'].append(('wait', self._all_waits('sp'), None))

    def emit(self):
        nc = self.nc
        with nc.Block() as block:
            def run(ename):
                def body(eng):
                    for kind, waits, payload in self.prog[ename]:
                        for sk, val in waits:
                            eng.wait_ge(self.semh[sk], val)
                        if kind == 'op':
                            ins = payload(eng)
                            ins.then_inc(self.semh['s_' + ename], 1)
                        elif kind == 'dma':
                            out, in_, sk = payload
                            eng.dma_start(out=out, in_=in_).then_inc(self.semh[sk], 16)
                return body
            block.tensor(run('pe'))
            block.scalar(run('act'))
            block.vector(run('dve'))
            block.gpsimd(run('pool'))
            block.sync(run('sp'))


def _consts():
    c = {}
    c['identb'] = np.eye(128, dtype=np.float32).astype(bf)
    c['identf'] = np.eye(128, dtype=np.float32)
    k = np.arange(128)[:, None]
    q = np.arange(128)[None, :]
    c['tri'] = (k <= q).astype(np.float32).astype(bf)
    c['atri'] = (k > q).astype(np.float32).astype(bf)
    c['U'] = (k <= q).astype(np.float32)
    c['onesf'] = np.ones((128, 128), np.float32)
    R = np.zeros((128, 128), np.float32)
    for m in range(128):
        if m % 64 < 32:
            R[m + 32, m] = -1.0
        else:
            R[m - 32, m] = 1.0
    c['R'] = R.astype(bf)
    half = 32
    inv = (10000.0 ** (-np.arange(half, dtype=np.float32) / half)).astype(np.float32)
    pos = np.arange(S_LEN, dtype=np.float32)
    ang = (pos[None, :] * inv[:, None]).astype(np.float32)
    c['cosT'] = np.tile(np.cos(ang).astype(np.float32), (4, 1))
    c['sinT'] = np.tile(np.sin(ang).astype(np.float32), (4, 1))
    posc = (np.arange(256, dtype=np.float32) * 16 + 31)
    angc = (posc[None, :] * inv[:, None]).astype(np.float32)
    c['cosC'] = np.tile(np.cos(angc).astype(np.float32), (4, 1))
    c['sinC'] = np.tile(np.sin(angc).astype(np.float32), (4, 1))
    s = np.arange(S_LEN)
    c['E'] = (s[None, :] // 64 == np.arange(64)[:, None]).astype(np.float32).astype(bf)
    cc = np.arange(256)
    vis = ((16 * cc[:, None] + 31 <= s[None, :]) & (cc[:, None] < 255)).astype(np.float32)
    c['vis'] = np.ascontiguousarray(vis.reshape(2, 128, S_LEN).transpose(1, 0, 2)).astype(bf)
    j = np.arange(64)
    ovl = ((16 * cc[:, None] < 64 * j[None, :] + 64) & (16 * cc[:, None] + 32 > 64 * j[None, :]) & (cc[:, None] < 255)).astype(np.float32)
    c['ovl'] = np.ascontiguousarray(ovl.reshape(2, 128, 64).transpose(1, 0, 2)).astype(bf)
    cur = s // 64
    valid = (j[None, :] <= cur[:, None])
    forced = (j[None, :] == 0) | (j[None, :] == cur[:, None]) | (j[None, :] == cur[:, None] - 1)
    bonus = np.where(valid, np.where(forced, 1e4, 0.0), -1.0).astype(np.float32)
    c['validm'] = np.ascontiguousarray(valid.astype(np.float32).reshape(32, 128, 64).transpose(1, 0, 2))
    c['bonus'] = np.ascontiguousarray(bonus.reshape(32, 128, 64).transpose(1, 0, 2))
    return c


IN_SPECS = [
    ('x', (S_LEN, D), F32), ('w_in', (D, 2848), F32), ('w_out', (D, D), F32), ('w_up', (D, 4096), F32),
    ('w_down', (4096, D), F32), ('g_attn', (1, D), F32), ('g_mlp', (1, D), F32), ('g_final', (1, D), F32),
    ('g_fox', (1, 512), F32), ('g_nsa', (1, 512), F32), ('b_f', (1, 8), F32), ('b_gate', (1, 24), F32),
    ('cmpk_peT', (64, 32), F32), ('cmpk_w1', (2048, 256), F32), ('cmpk_b1', (128, 2), F32), ('cmpk_w2', (256, 64), F32),
    ('cmpk_b2', (64, 1), F32),
    ('cmpv_peT', (64, 32), F32), ('cmpv_w1', (2048, 256), F32), ('cmpv_b1', (128, 2), F32), ('cmpv_w2', (256, 64), F32),
    ('cmpv_b2', (1, 64), F32),
    ('identb', (128, 128), BF16), ('identf', (128, 128), F32), ('tri', (128, 128), BF16), ('atri', (128, 128), BF16),
    ('U', (128, 128), F32), ('onesf', (128, 128), F32), ('R', (128, 128), BF16),
    ('cosT', (128, S_LEN), F32), ('sinT', (128, S_LEN), F32), ('cosC', (128, 256), F32), ('sinC', (128, 256), F32),
    ('E', (64, S_LEN), BF16), ('vis', (128, 2, S_LEN), BF16), ('ovl', (128, 2, 64), BF16),
    ('validm', (128, 32, 64), F32), ('bonus', (128, 32, 64), F32),
]


def build(phases=(1, 2, 3, 4), dbg=False):
    nc = bass.Bass("TRN2", target_bir_lowering=False)
    I = {}
    for name, shape, dt in IN_SPECS:
        I[name] = nc.dram_tensor(name, list(shape), dt, kind="ExternalInput").ap()
    out_d = nc.dram_tensor("out", [S_LEN, D], F32, kind="ExternalOutput").ap()
    skind = "ExternalOutput" if dbg else "Internal"
    FM = nc.dram_tensor("FM", [2048, S_LEN], BF16, kind=skind).ap()
    OS = nc.dram_tensor("OS", [S_LEN, D], F32, kind=skind).ap()
    DBG = nc.dram_tensor("DBG", [128, 2048], F32, kind=skind).ap()

    S = Sched(nc, ndma=LIM.get("ndma", 40))
    es_all = ExitStack()

    def sbt(es, name, shape, dt):
        return es.enter_context(nc.sbuf_tensor("sb_" + name, list(shape), dt)).ap()

    def pst(es, name, dt=F32):
        return es.enter_context(nc.psum_tensor("ps_" + name, [128, 2048 // (4 if dt == F32 else 2)], dt)).ap()

    def mm(out, lhsT, rhs, start=True, stop=True, r=(), w=()):
        S.op('pe', lambda e: e.matmul(out, lhsT=lhsT, rhs=rhs, start=start, stop=stop), reads=r, writes=w)

    def tp(out, in_, ident, r=(), w=()):
        S.op('pe', lambda e: e.transpose(out=out, in_=in_, identity=ident), reads=r, writes=w)

    def act(out, in_, func, bias=0.0, scale=1.0, r=(), w=()):
        S.op('act', lambda e: e.activation(out=out, in_=in_, func=func, bias=bias, scale=scale), reads=r, writes=w)

    def cp(eng, out, in_, r=(), w=()):
        if eng == 'act':
            S.op('act', lambda e: e.copy(out=out, in_=in_), reads=r, writes=w)
        else:
            S.op(eng, lambda e: e.tensor_copy(out=out, in_=in_), reads=r, writes=w)

    def tt(eng, out, in0, in1, op, r=(), w=()):
        S.op(eng, lambda e: e.tensor_tensor(out=out, in0=in0, in1=in1, op=op), reads=r, writes=w)

    def ts(eng, out, in0, s1, op0, s2=None, op1=None, r=(), w=()):
        if op1 is None:
            S.op(eng, lambda e: e.tensor_scalar(out=out, in0=in0, scalar1=s1, scalar2=None, op0=op0), reads=r, writes=w)
        else:
            S.op(eng, lambda e: e.tensor_scalar(out=out, in0=in0, scalar1=s1, scalar2=s2, op0=op0, op1=op1), reads=r, writes=w)

    def stt(eng, out, in0, sc, in1, op0, op1, r=(), w=()):
        S.op(eng, lambda e: e.scalar_tensor_tensor(out=out, in0=in0, scalar=sc, in1=in1, op0=op0, op1=op1), reads=r, writes=w)

    def red(eng, out, in_, op, r=(), w=()):
        S.op(eng, lambda e: e.tensor_reduce(out=out, in_=in_, axis=AX.X, op=op), reads=r, writes=w)

    def recip(out, in_, r=(), w=()):
        S.op('dve', lambda e: e.reciprocal(out=out, in_=in_), reads=r, writes=w)

    def mset(eng, ap, val, w=()):
        S.op(eng, lambda e: e.memset(ap, val), writes=w)

    def dma(out, in_, r=(), w=(), q='sp'):
        S.dma(q, out, in_, reads=r, writes=w)

    def rmsstat(x_ap, n, sq, ss, rstd, rk, tag):
        tt('dve', sq, x_ap, x_ap, ALU.mult, r=rk, w=[tag + 'sq'])
        red('dve', ss, sq, ALU.add, r=[tag + 'sq'], w=[tag + 'ss'])
        act(rstd, ss, AF.Ln, bias=epsb[:, 0:1], scale=1.0 / n, r=[tag + 'ss', 'epsb'], w=[tag + 'rstd'])
        act(rstd, rstd, AF.Exp, scale=-0.5, r=[tag + 'rstd'], w=[tag + 'rstd'])

    P = es_all
    identb = sbt(P, "identb", [128, 128], BF16)
    identf = sbt(P, "identf", [128, 128], F32)
    tri = sbt(P, "tri", [128, 128], BF16)
    atri = sbt(P, "atri", [128, 128], BF16)
    epsb = sbt(P, "epsb", [128, 2], F32)
    for nm, t in (('identb', identb), ('identf', identf), ('tri', tri), ('atri', atri)):
        dma(t, I[nm], w=[nm])
    mset('dve', epsb[:, 0:1], EPS, w=['epsb'])
    mset('dve', epsb[:, 1:2], 1.0, w=['epsb'])

    if 1 in phases or 2 in phases or 3 in phases:
        es12 = ExitStack()
        VF = sbt(es12, "VF", [128, NT, 8, 65], BF16)
        logf = sbt(es12, "logf", [128, NT, 8], F32)
        gates = sbt(es12, "gates", [128, NT, 24], F32)
        negc = sbt(es12, "negc", [128, NT, 8], F32)
        chi = sbt(es12, "chi", [8, S_LEN], BF16)
        clo = sbt(es12, "clo", [8, S_LEN], BF16)
        esV = ExitStack()
        VS = sbt(esV, "VS", [128, NT, 2, 65], BF16)
        VW = sbt(esV, "VW", [128, NT, 2, 65], BF16)
        mset('pool', logf.rearrange("p a b -> p (a b)"), 0.0, w=['logf'])
        mset('pool', gates.rearrange("p a b -> p (a b)"), 0.0, w=['gates'])
        mset('pool', VF.rearrange("p a b c -> p (a b c)"), 1.0, w=['VF'])
        mset('pool', VS.rearrange("p a b c -> p (a b c)"), 1.0, w=['VS'])
        mset('pool', VW.rearrange("p a b c -> p (a b c)"), 1.0, w=['VW'])

    if 1 in phases:
        with ExitStack() as es:
            Wfm = sbt(es, "Wfm", [128, 8, 2048], BF16)
            Wtm = sbt(es, "Wtm", [128, 8, 800], BF16)
            wst = [sbt(es, f"wst{i}", [128, 2848], F32) for i in range(2)]
            gat = sbt(es, "gat", [128, D], F32)
            bfb = sbt(es, "bfb", [128, 8], F32)
            bgb = sbt(es, "bgb", [128, 24], F32)
            Rm = sbt(es, "Rm", [128, 128], BF16)
            Um = sbt(es, "Um", [128, 128], F32)
            On = sbt(es, "On", [128, 128], F32)
            xt = [sbt(es, f"xt{i}", [128, D], F32) for i in range(2)]
            sq = sbt(es, "sq", [128, D], F32)
            ss = sbt(es, "ss", [128, 1], F32)
            rstd = sbt(es, "rstd", [128, 1], F32)
            hnb = [sbt(es, f"hnb{i}", [128, D], BF16) for i in range(2)]
            hnT = [sbt(es, f"hnT{i}", [128, 8, 512], BF16) for i in range(2)]
            fmo = [sbt(es, f"fmo{i}", [128, 512], BF16) for i in range(4)]
            cs = [sbt(es, f"cs{i}", [128, 2, 512], F32) for i in range(2)]
            xb = [sbt(es, f"xb{i}", [128, 512], BF16) for i in range(2)]
            t1 = [sbt(es, f"t1{i}", [128, 512], F32) for i in range(2)]
            t2 = [sbt(es, f"t2{i}", [128, 512], F32) for i in range(2)]
            zt = sbt(es, "zt", [128, 32], F32)
            Xp = sbt(es, "Xp", [128, NT, 8], F32)
            psT = pst(es, "psT", BF16)
            pfm = [pst(es, f"pfm{i}") for i in range(2)]
            prr = pst(es, "prr")
            ptA = [pst(es, f"ptA{i}") for i in range(2)]
            ptB = [pst(es, f"ptB{i}") for i in range(2)]
            pcc = ptA[0]

            dma(gat, I['g_attn'].partition_broadcast(128), w=['gat'])
            dma(bfb, I['b_f'].partition_broadcast(128), w=['bfb'])
            dma(bgb, I['b_gate'].partition_broadcast(128), w=['bgb'])
            dma(Rm, I['R'], w=['Rm'])
            dma(Um, I['U'], w=['Um'])
            dma(On, I['onesf'], w=['On'])
            fm_src = [(0, 1024, 0), (1544, 2056, 1024), (2056, 2312, 1536), (2312, 2440, 1792), (2568, 2696, 1920)]
            tm_src = [(1024, 1536, 0), (2440, 2568, 512), (2696, 2824, 640), (1536, 1544, 768), (2824, 2848, 776)]
            pieces = [('Wfm', 0, 512, 0, 'pool'), ('Wfm', 512, 1024, 512, 'dve'), ('Wfm', 1544, 2056, 1024, 'act'),
                      ('Wfm', 2056, 2312, 1536, 'dve'), ('Wfm', 2312, 2440, 1792, 'act'), ('Wfm', 2568, 2696, 1920, 'act'),
                      ('Wtm', 1024, 1536, 0, 'pool'), ('Wtm', 2440, 2568, 512, 'act'), ('Wtm', 2696, 2824, 640, 'dve'),
                      ('Wtm', 1536, 1544, 768, 'dve'), ('Wtm', 2824, 2848, 776, 'dve')]
            for kc in range(8):
                st = wst[kc % 2]
                dma(st, I['w_in'][kc * 128:(kc + 1) * 128, :], w=[('wst', kc % 2)])
                for pi, (wn, a_, b_, d0, eng) in enumerate(pieces):
                    dst = (Wfm if wn == 'Wfm' else Wtm)[:, kc, d0:d0 + (b_ - a_)]
                    S.op(eng, (lambda e, dst=dst, src=st[:, a_:b_]: e.copy(out=dst, in_=src)) if eng == 'act'
                         else (lambda e, dst=dst, src=st[:, a_:b_]: e.tensor_copy(out=dst, in_=src)),
                         reads=[('wst', kc % 2)], writes=[(wn, kc, pi)])
            ROPE = {8, 9, 10, 11, 14, 15}
            NCH = LIM.get('p1', 8)
            cnt = {'fi': 0, 'ri': 0}

            def cs_load(ci):
                c_s = ci % 2
                dma(cs[c_s][:, 0, :], I['cosT'][:, ci * 512:(ci + 1) * 512], w=[('cs', c_s, 0)])
                dma(cs[c_s][:, 1, :], I['sinT'][:, ci * 512:(ci + 1) * 512], w=[('cs', c_s, 1)])

            def prologue_tile(ci, t4):
                c_s = ci % 2
                kt = ci * 4 + t4
                s2 = kt % 2
                dma(xt[s2], I['x'][kt * 128:(kt + 1) * 128, :], w=[('xt', s2)])
                rmsstat(xt[s2], D, sq, ss, rstd, [('xt', s2)], 'p1')
                stt('dve', hnb[s2], xt[s2], rstd[:, 0:1], gat, ALU.mult, ALU.mult, r=[('xt', s2), 'p1rstd', 'gat'], w=[('hnb', s2)])
                for kc in range(8):
                    tp(psT[:, kc * 128:(kc + 1) * 128], hnb[s2][:, kc * 128:(kc + 1) * 128], identb, r=[('hnb', s2), 'identb'], w=['psT'])
                cp('act', hnT[c_s][:, :, t4 * 128:(t4 + 1) * 128], psT.rearrange("p (k t) -> p k t", k=8), r=['psT'], w=[('hnT', c_s)])

            if NCH > 0:
                cs_load(0)
                for t4 in range(4):
                    prologue_tile(0, t4)
            S.barrier()
            for ci in range(NCH):
                c_s = ci % 2
                if ci + 1 < NCH:
                    cs_load(ci + 1)
                pend_rot = []

                def rot_part2(pb_unused, r2, fo, fk, mt, ci=ci, c_s=c_s):
                    mm(prr, Rm, xb[r2], r=['Rm', ('xb', r2)], w=['prr'])
                    tt('dve', t2[r2], prr, cs[c_s][:, 1, :], ALU.mult, r=['prr', ('cs', c_s, 1)], w=[('t2', r2)])
                    tt('pool', fo, t1[r2], t2[r2], ALU.add, r=[('t1', r2), ('t2', r2)], w=[fk])
                    dma(FM[mt * 128:(mt + 1) * 128, ci * 512:(ci + 1) * 512], fo, r=[fk], w=[('FM', mt, ci)], q='pool')
                for mt in range(16):
                    pb = pfm[mt % 2]
                    pk = ('pfm', mt % 2)
                    for kc in range(8):
                        mm(pb, Wfm[:, kc, mt * 128:(mt + 1) * 128], hnT[c_s][:, kc, :], start=(kc == 0), stop=(kc == 7),
                           r=['Wfm', ('hnT', c_s)], w=[pk])
                    while pend_rot:
                        rot_part2(*pend_rot.pop(0))
                    fo = fmo[cnt['fi'] % 4]
                    fk = ('fmo', cnt['fi'] % 4)
                    cnt['fi'] += 1
                    if mt not in ROPE:
                        cp('act', fo, pb, r=[pk], w=[fk])
                        dma(FM[mt * 128:(mt + 1) * 128, ci * 512:(ci + 1) * 512], fo, r=[fk], w=[('FM', mt, ci)], q='act')
                    else:
                        r2 = cnt['ri'] % 2
                        cnt['ri'] += 1
                        cp('act', xb[r2], pb, r=[pk], w=[('xb', r2)])
                        tt('dve', t1[r2], pb, cs[c_s][:, 0, :], ALU.mult, r=[pk, ('cs', c_s, 0)], w=[('t1', r2)])
                        pend_rot.append((None, r2, fo, fk, mt))
                    if ci + 1 < NCH and mt in (3, 7, 11, 15):
                        prologue_tile(ci + 1, mt // 4)
                while pend_rot:
                    rot_part2(*pend_rot.pop(0))
                for t4 in range(4):
                    kt = ci * 4 + t4
                    pA_ = ptA[t4 % 2]
                    pB_ = ptB[t4 % 2]
                    ka = ('ptA', t4 % 2)
                    kb = ('ptB', t4 % 2)
                    for kc in range(8):
                        mm(pA_, hnT[c_s][:, kc, t4 * 128:(t4 + 1) * 128], Wtm[:, kc, 0:512], start=(kc == 0), stop=(kc == 7),
                           r=['Wtm', ('hnT', c_s)], w=[ka])
                    for kc in range(8):
                        mm(pB_[:, 0:288], hnT[c_s][:, kc, t4 * 128:(t4 + 1) * 128], Wtm[:, kc, 512:800], start=(kc == 0), stop=(kc == 7),
                           r=['Wtm', ('hnT', c_s)], w=[kb])
                    cp('act', VF[:, kt, :, 0:64], pA_.rearrange("p (h d) -> p h d", h=8), r=[ka], w=['VF'])
                    cp('act', VS[:, kt, :, 0:64], pB_[:, 0:128].rearrange("p (h d) -> p h d", h=2), r=[kb], w=['VS'])
                    cp('act', VW[:, kt, :, 0:64], pB_[:, 128:256].rearrange("p (h d) -> p h d", h=2), r=[kb], w=['VW'])
                    tt('dve', zt[:, 0:8], pB_[:, 256:264], bfb, ALU.add, r=[kb, 'bfb'], w=['zt'])
                    tt('dve', zt[:, 8:32], pB_[:, 264:288], bgb, ALU.add, r=[kb, 'bgb'], w=['zt'])
                    act(zt, zt, AF.Exp, scale=-1.0, r=['zt'], w=['zt'])
                    act(logf[:, kt, :], zt[:, 0:8], AF.Ln, bias=epsb[:, 1:2], r=['zt', 'epsb'], w=['logf'])
                    ts('dve', logf[:, kt, :], logf[:, kt, :], -1.0, ALU.mult, r=['logf'], w=['logf'])
                    ts('dve', zt[:, 8:32], zt[:, 8:32], 1.0, ALU.add, r=['zt'], w=['zt'])
                    recip(gates[:, kt, :], zt[:, 8:32], r=['zt'], w=['gates'])
            mset('dve', Xp[:, 0, :], 0.0, w=['Xp'])
            for kt in range(1, NT if LIM.get('stage', 9) >= 3 else 1):
                tt('dve', Xp[:, kt, :], Xp[:, kt - 1, :], logf[:, kt - 1, :], ALU.add, r=['Xp', 'logf'], w=['Xp'])
            if LIM.get('stage', 9) < 4:
                S.finish(); S.emit(); return nc
            lf2 = logf.rearrange("p k h -> p (k h)")
            xp2 = Xp.rearrange("p k h -> p (k h)")
            mm(pcc[:, 0:256], Um, lf2, start=True, stop=False, r=['Um', 'logf'], w=[('ptA', 0)])
            mm(pcc[:, 0:256], On, xp2, start=False, stop=True, r=['On', 'Xp'], w=[('ptA', 0)])
            ts('dve', negc.rearrange("p k h -> p (k h)"), pcc[:, 0:256], -1.0, ALU.mult, r=[('ptA', 0)], w=['negc'])
            for kg in range(8):
                pb = pfm[kg % 2]
                pk = ('pfm', kg % 2)
                for j4 in range(4):
                    kt = kg * 4 + j4
                    mm(pb[0:8, j4 * 128:(j4 + 1) * 128], logf[:, kt, :], Um, start=True, stop=False, r=['Um', 'logf'], w=[pk])
                    mm(pb[0:8, j4 * 128:(j4 + 1) * 128], Xp[:, kt, :], On, start=False, stop=True, r=['On', 'Xp'], w=[pk])
                ts('dve', chi[:, kg * 512:(kg + 1) * 512], pb[0:8, :], 8.0, ALU.mult, r=[pk], w=['chi'])
                stt('dve', clo[:, kg * 512:(kg + 1) * 512], pb[0:8, :], 8.0, chi[:, kg * 512:(kg + 1) * 512], ALU.mult, ALU.subtract,
                    r=[pk, 'chi'], w=['clo'])
            if dbg:
                dma(DBG[:, 0:256], negc.rearrange("p k h -> p (k h)"), r=['negc'])
                dma(DBG[:, 256:1024], gates.rearrange("p k h -> p (k h)"), r=['gates'])
        S.barrier()

    def finalize(po, osb, osk, ptr, ptrk, rz, otok, otk, gate_ap=None, ev='act'):
        cp(ev, osb[0:65, :], po[0:65, :], r=[osk[0]], w=[osk[1]])
        for j in range(4):
            tp(ptr[:, j * 65:(j + 1) * 65], osb[0:65, j * 128:(j + 1) * 128], identf[0:65, 0:65], r=[osk[1], 'identf'], w=[ptrk])
        pv = ptr[:, 0:260].rearrange("p (j d) -> p j d", j=4)
        recip(rz[:, 0:4], pv[:, :, 64], r=[ptrk], w=['rz'])
        if gate_ap is not None:
            tt('dve', rz[:, 0:4], rz[:, 0:4], gate_ap, ALU.mult, r=['rz', 'gates'], w=['rz'])
        tt('dve', otok, pv[:, :, 0:64], rz[:, 0:4].unsqueeze(2).broadcast_to([128, 4, 64]), ALU.mult, r=[ptrk, 'rz'], w=[otk])

    if 3 in phases:
        with ExitStack() as es:
            KSA = [sbt(es, f"KSA{g}", [128, S_LEN], BF16) for g in range(2)]
            KW = [sbt(es, f"KW{g}", [64, S_LEN], BF16) for g in range(2)]
            KCT = [sbt(es, f"KCT{g}", [64, 256], BF16) for g in range(2)]
            VOZ = [sbt(es, f"VOZ{g}", [128, 2, 129], BF16) for g in range(2)]
            vis = sbt(es, "vis", [128, 2, S_LEN], BF16)
            validm = sbt(es, "validm", [128, 32, 64], F32)
            bonus = sbt(es, "bonus", [128, 32, 64], F32)
            Rm = sbt(es, "Rm3", [128, 128], BF16)
            for g in range(2):
                dma(KSA[g][0:64, :], FM[1792 + g * 64:1792 + (g + 1) * 64, :], r=['FM'], w=[('KSA', g)])
                dma(KSA[g][64:128, :], I['E'], w=[('KSAe', g)])
                dma(KW[g], FM[1920 + g * 64:1920 + (g + 1) * 64, :], r=['FM'], w=[('KW', g)])
                mset('pool', VOZ[g], 0.0, w=[('VOZ', g)])
            dma(Rm, I['R'], w=['Rm'])
            with ExitStack() as ec:
                XC = sbt(ec, "XC", [128, S_LEN], BF16)
                W1 = sbt(ec, "W1", [128, 32, 256], BF16)
                W1s2 = [sbt(ec, f"W1s{i}", [128, 8, 256], F32) for i in range(2)]
                W2s = sbt(ec, "W2s", [128, 2, 64], F32)
                W2 = sbt(ec, "W2", [128, 2, 64], BF16)
                peT = sbt(ec, "peT", [128, 32], F32)
                peTb = sbt(ec, "peTb", [128, 32], BF16)
                b1 = sbt(ec, "b1", [128, 2], F32)
                btot = sbt(ec, "btot", [128, 2], F32)
                b2c = sbt(ec, "b2c", [64, 1], F32)
                b2r = sbt(ec, "b2r", [128, 64], F32)
                hT = [sbt(ec, f"hT{i}", [128, 256], BF16) for i in range(4)]
                xg = sbt(ec, "xg", [128, 256], F32)
                ug = sbt(ec, "ug", [128, 256], F32)
                kx = sbt(ec, "kx", [64, 256], BF16)
                cC = sbt(ec, "cC", [64, 2, 256], F32)
                k1 = sbt(ec, "k1", [64, 256], F32)
                k2 = sbt(ec, "k2", [64, 256], F32)
                ph = [pst(ec, f"ph{i}") for i in range(2)]
                pk2 = pst(ec, "pk2")
                pb1 = pst(ec, "pb1")
                dma(cC[:, 0, :], I['cosC'][0:64, :], w=['cC0'])
                dma(cC[:, 1, :], I['sinC'][0:64, :], w=['cC1'])
                for which in ('k', 'v'):
                    pre = 'cmp' + which
                    dma(XC, FM[1536:1664, :] if which == 'k' else FM[1664:1792, :], r=['FM'], w=['XC'])
                    w1v = I[pre + '_w1'].rearrange("(l d) h -> d l h", d=64)
                    for lq in range(4):
                        W1s = W1s2[lq % 2]
                        for half in range(2):
                            dma(W1s[half * 64:(half + 1) * 64, :, :], w1v[:, lq * 8:(lq + 1) * 8, :], w=[('W1s', lq % 2, half)])
                        cp('pool' if lq % 2 == 0 else 'dve', W1[:, lq * 8:(lq + 1) * 8, :], W1s, r=[('W1s', lq % 2, 0), ('W1s', lq % 2, 1)], w=[('W1', lq)])
                    dma(W2s, I[pre + '_w2'].rearrange("(c p) d -> p c d", p=128), w=['W2s'])
                    cp('pool', W2, W2s, r=['W2s'], w=['W2'])
                    for half in range(2):
                        dma(peT[half * 64:(half + 1) * 64, :], I[pre + '_peT'], w=[('peT', half)])
                    cp('pool', peTb, peT, r=[('peT', 0), ('peT', 1)], w=['peTb'])
                    dma(b1, I[pre + '_b1'], w=['b1'])
                    if which == 'k':
                        dma(b2c, I['cmpk_b2'], w=['b2c'])
                    else:
                        dma(b2r, I['cmpv_b2'].partition_broadcast(128), w=['b2r'])
                    for hc in range(2):
                        for l in range(32):
                            mm(pb1[:, hc:hc + 1], W1[0:64, l, hc * 128:(hc + 1) * 128], peTb[0:64, l:l + 1], start=(l == 0), stop=(l == 31),
                               r=[('W1', 0), ('W1', 1), ('W1', 2), ('W1', 3), 'peTb'], w=['pb1'])
                    tt('dve', btot, pb1[:, 0:2], b1, ALU.add, r=['pb1', 'b1'], w=['btot'])
                    xcv = XC.rearrange("p (c s) -> p c s", s=16)
                    for g in range(2):
                        for hc in range(2):
                            pp = ph[hc]
                            for l in range(32):
                                rhs = xcv[g * 64:(g + 1) * 64, 0:255, l] if l < 16 else xcv[g * 64:(g + 1) * 64, 1:256, l - 16]
                                mm(pp[:, 0:255], W1[g * 64:(g + 1) * 64, l, hc * 128:(hc + 1) * 128], rhs, start=(l == 0), stop=(l == 31),
                                   r=[('W1', 0), ('W1', 1), ('W1', 2), ('W1', 3), 'XC'], w=[('ph', hc)])
                            hh = hT[g * 2 + hc]
                            hk = ('hT', g * 2 + hc)
                            ts('dve', xg[:, 0:255], pp[:, 0:255], btot[:, hc:hc + 1], ALU.add, r=[('ph', hc), 'btot'], w=['xg'])
                            tt('dve', ug[:, 0:255], xg[:, 0:255], xg[:, 0:255], ALU.mult, r=['xg'], w=['ug'])
                            ts('dve', ug[:, 0:255], ug[:, 0:255], 0.044715, ALU.mult, 1.0, ALU.add, r=['ug'], w=['ug'])
                            tt('dve', ug[:, 0:255], ug[:, 0:255], xg[:, 0:255], ALU.mult, r=['ug', 'xg'], w=['ug'])
                            act(ug[:, 0:255], ug[:, 0:255], AF.Exp, scale=-1.59576912, r=['ug'], w=['ug'])
                            ts('dve', ug[:, 0:255], ug[:, 0:255], 1.0, ALU.add, r=['ug'], w=['ug'])
                            recip(ug[:, 0:255], ug[:, 0:255], r=['ug'], w=['ug'])
                            tt('dve', hh[:, 0:255], xg[:, 0:255], ug[:, 0:255], ALU.mult, r=['ug', 'xg'], w=[hk])
                        if which == 'k':
                            for hc in range(2):
                                mm(pk2[0:64, 0:255], W2[:, hc, :], hT[g * 2 + hc][:, 0:255], start=(hc == 0), stop=(hc == 1),
                                   r=['W2', ('hT', g * 2 + hc)], w=['pk2'])
                            ts('dve', k1[:, 0:255], pk2[0:64, 0:255], b2c[:, 0:1], ALU.add, r=['pk2', 'b2c'], w=['k1'])
                            cp('dve', kx[:, 0:255], k1[:, 0:255], r=['k1'], w=['kx'])
                            mm(pk2[0:64, 256:511], Rm[0:64, 0:64], kx[:, 0:255], r=['Rm', 'kx'], w=['pk2'])
                            tt('dve', k2[:, 0:255], pk2[0:64, 256:511], cC[:, 1, 0:255], ALU.mult, r=['pk2', 'cC1'], w=['k2'])
                            tt('dve', k1[:, 0:255], k1[:, 0:255], cC[:, 0, 0:255], ALU.mult, r=['k1', 'cC0'], w=['k1'])
                            tt('dve', KCT[g][:, 0:255], k1[:, 0:255], k2[:, 0:255], ALU.add, r=['k1', 'k2'], w=[('KCT', g)])
                        else:
                            for ct in range(2):
                                M = 128 if ct == 0 else 127
                                for hc in range(2):
                                    mm(pk2[0:M, ct * 64:(ct + 1) * 64], hT[g * 2 + hc][:, ct * 128:ct * 128 + M], W2[:, hc, :],
                                       start=(hc == 0), stop=(hc == 1), r=['W2', ('hT', g * 2 + hc)], w=['pk2'])
                                tt('dve', VOZ[g][0:M, ct, 0:64], pk2[0:M, ct * 64:(ct + 1) * 64], b2r[0:M, :], ALU.add,
                                   r=['pk2', 'b2r'], w=[('VOZ', g)])
                dma(vis, I['vis'], w=['vis'])
                dma(validm, I['validm'], w=['validm'])
                dma(bonus, I['bonus'], w=['bonus'])
                for g in range(2):
                    dma(VOZ[g][:, :, 64:128], I['ovl'], w=[('VOZo', g)])
                    mset('pool', VOZ[g][:, :, 128:129], 1.0, w=[('VOZ1', g)])
            S.barrier()
            QA = [sbt(es, f"QA{i}", [128, 4, 128], BF16) for i in range(2)]
            pcm = [sbt(es, f"pcm{i}", [128, 512], BF16) for i in range(2)]
            pt = [sbt(es, f"ptn{i}", [128, 512], BF16) for i in range(3)]
            osb = sbt(es, "osb3", [65, 512], F32)
            rz = sbt(es, "rz3", [128, 4], F32)
            rzA = sbt(es, "rzA", [128, 8], F32)
            ocmp = [sbt(es, f"ocmp{i}", [128, 4, 64], F32) for i in range(3)]
            oslc = sbt(es, "oslc", [128, 4, 64], F32)
            owin = [sbt(es, f"owin{i}", [128, 4, 64], F32) for i in range(2)]
            pendB = []
            pendN = []
            onsa = [sbt(es, f"onsa{i}", [128, 4, 64], F32) for i in range(2)]
            impw = sbt(es, "impw", [128, 4, 64], F32)
            score_t = sbt(es, "score", [128, 64], F32)
            tmpm = sbt(es, "tmpm", [128, 64], F32)
            m8 = sbt(es, "m8", [128, 16], F32)
            negm = sbt(es, "negm", [128, 128], BF16)
            pS = [pst(es, f"pS3{i}") for i in range(3)]
            pO = [pst(es, f"pO3{i}") for i in range(3)]
            pcz = [pst(es, f"pcz{i}") for i in range(2)]
            mset('pool', negm, 0.0, w=['negm'])
            vk = [('VOZ', 0), ('VOZ', 1), ('VOZo', 0), ('VOZo', 1), ('VOZ1', 0), ('VOZ1', 1)]
            items = [(g, qt) for g in range(2) for qt in range(LIM.get('p3q', 32))]

            def ctx(i):
                g, qt = items[i]
                s2 = i % 2
                qa = QA[s2]
                return g, qt, s2, qa, qa.rearrange("p h q -> p (h q)"), gates[:, qt, g * 12:(g + 1) * 12].rearrange("p (h b) -> p h b", b=3)

            def stageA1a(i):
                g, qt, s2, qa, qa2, gsl = ctx(i)
                dma(qa[0:64, :, :], FM[1024 + g * 256:1024 + (g + 1) * 256, qt * 128:(qt + 1) * 128].rearrange("(h d) q -> d h q", h=4),
                    r=['FM'], w=[('qa', s2, 'q')])
                nct = 1 if qt < 16 else 2
                for ct in range(nct):
                    M = 128 if ct == 0 else 127
                    mm(pcz[ct][0:M, :], KCT[g][:, ct * 128:ct * 128 + M], qa2[0:64, :], r=[('KCT', g), ('qa', s2, 'q')], w=[('pcz', ct)])
                    act(pcm[ct][0:M, :], pcz[ct][0:M, :], AF.Exp, scale=0.125, r=[('pcz', ct)], w=[('pcm', ct)])
                    tt('pool', pcm[ct][0:M, :].rearrange("p (h q) -> p h q", h=4), pcm[ct][0:M, :].rearrange("p (h q) -> p h q", h=4),
                       vis[0:M, ct, qt * 128:(qt + 1) * 128].unsqueeze(1).broadcast_to([M, 4, 128]), ALU.mult,
                       r=[('pcm', ct), 'vis'], w=[('pcm', ct)])

            def stageA1b(i):
                g, qt, s2, qa, qa2, gsl = ctx(i)
                nct = 1 if qt < 16 else 2
                oc = ocmp[i % 3]
                for h in range(4):
                    pz = pcz[h // 2]
                    for ct in range(nct):
                        M = 128 if ct == 0 else 127
                        mm(pz[:, (h % 2) * 256:(h % 2) * 256 + 129], pcm[ct][0:M, h * 128:(h + 1) * 128], VOZ[g][0:M, ct, :],
                           start=(ct == 0), stop=(ct == nct - 1), r=[('pcm', 0), ('pcm', 1)] + vk, w=[('pcz', h // 2)])
                for hb in range(2):
                    pzv = pcz[hb].rearrange("p (h c) -> p h c", h=2)
                    ts('dve', rzA[:, 2 * hb:2 * hb + 2], pzv[:, :, 128], 1e-30, ALU.max, r=[('pcz', hb)], w=['rzA'])
                recip(rzA[:, 0:4], rzA[:, 0:4], r=['rzA'], w=['rzA'])
                for hb in range(2):
                    pzv = pcz[hb].rearrange("p (h c) -> p h c", h=2)
                    tt('dve', impw[:, 2 * hb:2 * hb + 2, :], pzv[:, :, 64:128], rzA[:, 2 * hb:2 * hb + 2].unsqueeze(2).broadcast_to([128, 2, 64]),
                       ALU.mult, r=[('pcz', hb), 'rzA'], w=['impw'])
                tt('dve', rzA[:, 4:8], rzA[:, 0:4], gsl[:, :, 0], ALU.mult, r=['rzA', 'gates'], w=['rzA2'])
                for hb in range(2):
                    pzv = pcz[hb].rearrange("p (h c) -> p h c", h=2)
                    tt('dve', oc[:, 2 * hb:2 * hb + 2, :], pzv[:, :, 0:64], rzA[:, 4 + 2 * hb:6 + 2 * hb].unsqueeze(2).broadcast_to([128, 2, 64]),
                       ALU.mult, r=[('pcz', hb), 'rzA2'], w=[('ocmp', i % 3)])
                red('dve', score_t, impw.rearrange("p h j -> p j h"), ALU.add, r=['impw'], w=['score'])
                tt('dve', score_t, score_t, validm[:, qt, :], ALU.mult, r=['score', 'validm'], w=['score'])
                tt('dve', score_t, score_t, bonus[:, qt, :], ALU.add, r=['score', 'bonus'], w=['score'])
                S.op('dve', lambda e: e.max(out=m8[:, 0:8], in_=score_t), reads=['score'], writes=['m8'])
                S.op('dve', lambda e: e.match_replace(out=tmpm, in_to_replace=m8[:, 0:8], in_values=score_t, imm_value=-2.0),
                     reads=['score', 'm8'], writes=['tmpm'])
                S.op('dve', lambda e: e.max(out=m8[:, 8:16], in_=tmpm), reads=['tmpm'], writes=['m8b'])
                ts('dve', negm[:, 64:128], score_t, m8[:, 15:16], ALU.is_lt, NEGM, ALU.mult, r=['score', 'm8b'], w=['negm'])

            def stageA2(i):
                g, qt, s2, qa, qa2, gsl = ctx(i)
                pT = pcz[0].bitcast(BF16)
                tp(pT[:, 0:128], negm, identb, r=['negm', 'identb'], w=[('pcz', 0)])
                cp('dve', qa[64:128, :, :], pT[64:128, 0:128].unsqueeze(1).broadcast_to([64, 4, 128]), r=[('pcz', 0)], w=[('qa', s2, 'm')])

            def stageB(i):
                g, qt, s2, qa, qa2, gsl = ctx(i)
                qq = [('qa', s2, 'q')]
                qm = [('qa', s2, 'q'), ('qa', s2, 'm')]
                tiles = []
                w0 = max(0, qt - 4)
                for kt in range(w0, qt + 1):
                    tiles.append(('w', kt, kt == w0, kt == qt))
                for kt in range(qt + 1):
                    tiles.append(('s', kt, kt == 0, kt == qt))

                def score(n):
                    br, kt, first, last = tiles[n]
                    if br == 's':
                        mm(pS[n % 3], KSA[g][:, kt * 128:(kt + 1) * 128], qa2, r=[('KSA', g), ('KSAe', g)] + qm, w=[('pS', n % 3)])
                    else:
                        mm(pS[n % 3], KW[g][:, kt * 128:(kt + 1) * 128], qa2[0:64, :], r=[('KW', g)] + qq, w=[('pS', n % 3)])
                score(0)
                if len(tiles) > 1:
                    score(1)
                for n in range(len(tiles)):
                    br, kt, first, last = tiles[n]
                    if n + 2 < len(tiles):
                        score(n + 2)
                    if n == 1 and i + 1 < len(items):
                        stageA1b(i + 1)
                    if n == min(3, len(tiles) - 1) and pendN:
                        pendN.pop(0)()
                    b3 = n % 3
                    act(pt[b3], pS[b3], AF.Exp, scale=0.125, r=[('pS', b3)], w=[('pt', b3)])
                    p3 = pt[b3].rearrange("p (h q) -> p h q", h=4)
                    if kt == qt:
                        tt('pool', p3, p3, tri.unsqueeze(1).broadcast_to([128, 4, 128]), ALU.mult, r=[('pt', b3), 'tri'], w=[('pt', b3)])
                    if br == 'w' and kt == qt - 4:
                        tt('pool', p3, p3, atri.unsqueeze(1).broadcast_to([128, 4, 128]), ALU.mult, r=[('pt', b3), 'atri'], w=[('pt', b3)])
                    bi = (i % 2) if br == 's' else 2
                    V = VS if br == 's' else VW
                    mm(pO[bi][0:65, :], V[:, kt, g, :], pt[b3], start=first, stop=last, r=['VS', 'VW', ('pt', b3)], w=[('pO', bi)])
                    if last and br == 'w':
                        def fin_w(bi=bi, gsl=gsl, s2=s2):
                            finalize(pO[bi], osb, [('pO', bi), 'osb'], pO[bi], ('pO', bi), rz, owin[s2], ('owin', s2), gate_ap=gsl[:, :, 2])
                        pendB.append((n + 3, fin_w))
                    if last and br == 's':
                        def fin_s(bi=bi, gsl=gsl, s2=s2, g=g, qt=qt, i=i):
                            finalize(pO[bi], osb, [('pO', bi), 'osb'], pO[bi], ('pO', bi), rz, oslc, 'oslc', gate_ap=gsl[:, :, 1])
                            o2 = i % 2
                            tt('dve', onsa[o2], ocmp[i % 3], oslc, ALU.add, r=[('ocmp', i % 3), 'oslc'], w=[('onsa', o2)])
                            tt('dve', onsa[o2], onsa[o2], owin[s2], ALU.add, r=[('onsa', o2), ('owin', s2)], w=[('onsa', o2)])
                            dma(OS[qt * 128:(qt + 1) * 128, 512 + g * 256:512 + (g + 1) * 256], onsa[o2].rearrange("p h d -> p (h d)"),
                                r=[('onsa', o2)], w=[('OSn', g, qt)], q='sp')
                        pendN.append(fin_s)
                    while pendB and (pendB[0][0] <= n or n == len(tiles) - 1):
                        pendB.pop(0)[1]()

            stageA1a(0)
            stageA1b(0)
            stageA2(0)
            for i in range(len(items)):
                if i + 1 < len(items):
                    stageA1a(i + 1)
                stageB(i)
                if i + 1 < len(items):
                    stageA2(i + 1)
            while pendN:
                pendN.pop(0)()
        S.barrier()

    if 1 in phases or 2 in phases or 3 in phases:
        esV.close()
    esW = ExitStack()
    PREF = [False]
    if 2 in phases:
        with ExitStack() as es:
            QFA = [sbt(es, f"QFA{i}", [128, S_LEN], BF16) for i in range(2)]
            KFA = [sbt(es, f"KFA{i}", [128, S_LEN], BF16) for i in range(2)]
            pt = [sbt(es, f"pt{i}", [128, 512], BF16) for i in range(4)]
            osb = sbt(es, "osb", [65, 512], F32)
            rz = sbt(es, "rz", [128, 4], F32)
            otok = [sbt(es, f"otok{i}", [128, 4, 64], F32) for i in range(2)]
            pS = [pst(es, f"pS{i}") for i in range(4)]
            pO = [pst(es, f"pO{i}") for i in range(2)]
            ptr = pst(es, "ptr")
            if 4 in phases:
                Wu = nc.alloc_sbuf_tensor_at("sb_Wu", [128, 8 * 4096], BF16, offset=229376 - 64 - 65536).ap().rearrange("p (k n) -> p k n", k=8)
                Wo = nc.alloc_sbuf_tensor_at("sb_Wo", [128, 8 * D], BF16, offset=229376 - 64 - 65536 - 16384).ap().rearrange("p (k n) -> p k n", k=8)
                wstf = [sbt(es, f"wstf{i}", [128, 2048], F32) for i in range(2)]
                PREF[0] = True
                assert nc.sbuf_bytes_remaining >= 81984 + 256, nc.sbuf_bytes_remaining
            for i in range(2):
                mset('pool', QFA[i], 0.0, w=[('qfa', i, 'q'), ('qfa', i, 'h'), ('qfa', i, 'l')])
                mset('pool', KFA[i], 0.0, w=[('kfa', i)])
                mset('pool', KFA[i][64:65, :], 1.0, w=[('kfa', i)])
                mset('pool', KFA[i][96:97, :], 1.0, w=[('kfa', i)])
            oi_box = [0]
            pend = []
            for h in range(LIM.get('p2h', 8)):
                s2 = h % 2
                dma(QFA[s2][0:64, :], FM[h * 64:(h + 1) * 64, :], r=['FM'], w=[('qfa', s2, 'q')])
                dma(QFA[s2][64:65, :], chi[h:h + 1, :], r=['chi'], w=[('qfa', s2, 'h')])
                dma(QFA[s2][96:97, :], clo[h:h + 1, :], r=['clo'], w=[('qfa', s2, 'l')])
                dma(KFA[s2][0:64, :], FM[512 + h * 64:512 + (h + 1) * 64, :], r=['FM'], w=[('kfa', s2)])
                qk = [('qfa', s2, 'q'), ('qfa', s2, 'h'), ('qfa', s2, 'l')]
                if PREF[0]:
                    dma(wstf[0][:, 0:D], I['w_out'][h * 128:(h + 1) * 128, :], w=[('wstf', 0)])
                    cp('pool', Wo[:, h, :], wstf[0][:, 0:D], r=[('wstf', 0)], w=['Wo'])
                    for hf in range(2):
                        dma(wstf[1 - hf], I['w_up'][h * 128:(h + 1) * 128, hf * 2048:(hf + 1) * 2048], w=[('wstf', 1 - hf)])
                        cp('pool', Wu[:, h, hf * 2048:(hf + 1) * 2048], wstf[1 - hf], r=[('wstf', 1 - hf)], w=['Wu'])
                tiles = []
                for qi in range(LIM.get('p2q', 8)):
                    nkt = 4 * qi + 4
                    for kt in range(nkt):
                        j = kt - 4 * qi
                        tiles.append((qi, kt, 128 * j if j > 0 else 0, j >= 0, kt == 0, kt == nkt - 1))

                def score(n):
                    qi, kt, c0, dg, first, last = tiles[n]
                    mm(pS[n % 4][:, c0:512], KFA[s2][:, kt * 128:(kt + 1) * 128], QFA[s2][:, qi * 512 + c0:(qi + 1) * 512],
                       r=qk + [('kfa', s2)], w=[('pS', n % 4)])
                score(0)
                score(1)
                score(2)
                for n in range(len(tiles)):
                    qi, kt, c0, dg, first, last = tiles[n]
                    if n + 3 < len(tiles):
                        score(n + 3)
                    b3 = n % 4
                    act(pt[b3][:, c0:512], pS[b3][:, c0:512], AF.Exp, bias=negc[:, kt, h:h + 1], scale=0.125,
                        r=[('pS', b3), 'negc'], w=[('pt', b3)])
                    if dg:
                        tt('pool', pt[b3][:, c0:c0 + 128], pt[b3][:, c0:c0 + 128], tri, ALU.mult, r=[('pt', b3), 'tri'], w=[('pt', b3)])
                    po = pO[qi % 2]
                    mm(po[0:65, c0:512], VF[:, kt, h, :], pt[b3][:, c0:512], start=first, stop=last,
                       r=['VF', ('pt', b3)], w=[('pO', qi % 2)])
                    if last:
                        def fin_fox(po=po, qi=qi, h=h):
                            o2 = oi_box[0] % 2
                            oi_box[0] += 1
                            finalize(po, osb, [('pO', qi % 2), 'osb'], ptr, 'ptr', rz, otok[o2], ('otok', o2), ev='dve')
                            dma(OS[qi * 512:(qi + 1) * 512, h * 64:(h + 1) * 64].rearrange("(j p) d -> p j d", p=128), otok[o2],
                                r=[('otok', o2)], w=[('OS', h, qi)], q='sp')
                        pend.append((n + 3, fin_fox))
                    while pend and (pend[0][0] <= n or n == len(tiles) - 1):
                        pend.pop(0)[1]()
        S.barrier()

    if 1 in phases or 2 in phases or 3 in phases:
        es12.close()
        S.barrier()

    if 4 in phases:
        with ExitStack() as es:
            if not PREF[0]:
                Wo = sbt(es, "Wo", [128, 8, D], BF16)
                Wu = sbt(es, "Wu", [128, 8, 4096], BF16)
            Wd = sbt(es, "Wd", [128, 32, D], BF16)
            gfn = sbt(es, "gfn", [128, D], F32)
            gml = sbt(es, "gml", [128, D], F32)
            gfi = sbt(es, "gfi", [128, D], F32)
            with ExitStack() as ew:
                wst = [sbt(ew, f"wst4{i}", [128, 2048], F32) for i in range(4)]
                assert (not PREF[0]) or nc.sbuf_bytes_remaining >= 81984 + 256, nc.sbuf_bytes_remaining
                wi = 0
                for kc in range(0 if PREF[0] else 8):
                    st = wst[wi % 2]
                    dma(st[:, 0:D], I['w_out'][kc * 128:(kc + 1) * 128, :], w=[('wst', wi % 2)])
                    cp('pool', Wo[:, kc, :], st[:, 0:D], r=[('wst', wi % 2)], w=['Wo'])
                    wi += 1
                for kc in range(0 if PREF[0] else 8):
                    for hf in range(2):
                        st = wst[wi % 2]
                        dma(st, I['w_up'][kc * 128:(kc + 1) * 128, hf * 2048:(hf + 1) * 2048], w=[('wst', wi % 2)])
                        cp('pool' if hf == 0 else 'dve', Wu[:, kc, hf * 2048:(hf + 1) * 2048], st, r=[('wst', wi % 2)], w=['Wu'])
                        wi += 1
                for hc in range(0, 32, 2):
                    st = wst[2 + wi % 2] if (wi // 2) % 2 else wst[wi % 2]
                    sk4 = ('wst4', (2 + wi % 2) if (wi // 2) % 2 else wi % 2)
                    dma(st.rearrange("p (c d) -> p c d", c=2), I['w_down'][hc * 128:(hc + 2) * 128, :].rearrange("(c p) d -> p c d", p=128),
                        w=[sk4], q=('sp' if wi % 2 == 0 else 'act'))
                    cp('pool' if wi % 2 == 0 else 'dve', Wd[:, hc:hc + 2, :], st.rearrange("p (c d) -> p c d", c=2), r=[sk4],
                       w=[('Wd', 'pool' if wi % 2 == 0 else 'dve')])
                    wi += 1
                dma(gfn[:, 0:512], I['g_fox'].partition_broadcast(128), w=['gfn0'])
                dma(gfn[:, 512:1024], I['g_nsa'].partition_broadcast(128), w=['gfn1'])
                dma(gml, I['g_mlp'].partition_broadcast(128), w=['gml'])
                dma(gfi, I['g_final'].partition_broadcast(128), w=['gfi'])
            S.barrier()
            xh = [sbt(es, f"xh{i}", [128, D], F32) for i in range(2)]
            ot = [sbt(es, f"ot{i}", [128, D], F32) for i in range(2)]
            sq = sbt(es, "sq4", [128, D], F32)
            ss = sbt(es, "ss4", [128, 2], F32)
            rstd = sbt(es, "rstd4", [128, 2], F32)
            ssm = sbt(es, "ssm4", [128, 2], F32)
            rstdm = sbt(es, "rstdm4", [128, 2], F32)
            yb = sbt(es, "yb", [128, D], BF16)
            ya = sbt(es, "ya", [128, D], BF16)
            yT = sbt(es, "yT", [128, 8, 128], BF16)
            h1T = [sbt(es, f"h1T{i}", [128, 8, 128], BF16) for i in range(2)]
            rl = [sbt(es, f"rl{i}", [128, 128], F32) for i in range(2)]
            hid = sbt(es, "hid", [128, 32, 128], BF16)
            fin = [sbt(es, f"fin{i}", [128, D], F32) for i in range(2)]
            psT = pst(es, "psT4", BF16)
            pP = [pst(es, f"pP{i}") for i in range(2)]
            pA = [pst(es, f"pA{i}") for i in range(2)]
            pU = [pst(es, f"pU{i}") for i in range(3)]
            assert (not PREF[0]) or nc.sbuf_bytes_remaining >= 81984 + 256, nc.sbuf_bytes_remaining
            LIM['_rem4'] = nc.sbuf_bytes_remaining

            def p4load_o(kt):
                dma(ot[kt % 2], OS[kt * 128:(kt + 1) * 128, :], r=['OS'], w=[('ot', kt % 2)])

            def p4load_x(kt):
                dma(xh[kt % 2], I['x'][kt * 128:(kt + 1) * 128, :], w=[('xh', kt % 2)])

            def P_a(kt):
                s2 = kt % 2
                for gi in range(2):
                    tt('dve', sq[:, gi * 512:(gi + 1) * 512], ot[s2][:, gi * 512:(gi + 1) * 512], ot[s2][:, gi * 512:(gi + 1) * 512], ALU.mult,
                       r=[('ot', s2)], w=['sq'])
                red('dve', ss, sq.rearrange("p (g d) -> p g d", g=2), ALU.add, r=['sq'], w=['ss'])
                act(rstd, ss, AF.Ln, bias=epsb[:, 0:1], scale=1.0 / 512, r=['ss', 'epsb'], w=['rstd'])
                act(rstd, rstd, AF.Exp, scale=-0.5, r=['rstd'], w=['rstd'])
                for gi in range(2):
                    stt('dve', ya[:, gi * 512:(gi + 1) * 512], ot[s2][:, gi * 512:(gi + 1) * 512], rstd[:, gi:gi + 1], gfn[:, gi * 512:(gi + 1) * 512],
                        ALU.mult, ALU.mult, r=[('ot', s2), 'rstd', 'gfn0', 'gfn1'], w=['ya'])
                if kt + 2 < NT:
                    p4load_o(kt + 2)

            def P_a2(kt):
                for kc in range(8):
                    tp(psT[:, kc * 128:(kc + 1) * 128], ya[:, kc * 128:(kc + 1) * 128], identb, r=['ya', 'identb'], w=['psT'])
                cp('act', yT, psT.rearrange("p (k t) -> p k t", k=8), r=['psT'], w=['yT'])

            def P_b(kt):
                s2 = kt % 2
                for hf in range(2):
                    for kc in range(8):
                        mm(pP[hf], yT[:, kc, :], Wo[:, kc, hf * 512:(hf + 1) * 512], start=(kc == 0), stop=(kc == 7), r=['yT', 'Wo'], w=[('pP', hf)])
                    tt('dve', xh[s2][:, hf * 512:(hf + 1) * 512], pP[hf], xh[s2][:, hf * 512:(hf + 1) * 512], ALU.add,
                       r=[('pP', hf), ('xh', s2)], w=[('xh', s2)])
                tt('dve', sq, xh[s2], xh[s2], ALU.mult, r=[('xh', s2)], w=['sq'])
                red('dve', ss[:, 0:1], sq, ALU.add, r=['sq'], w=['ss'])
                act(rstd[:, 0:1], ss[:, 0:1], AF.Ln, bias=epsb[:, 0:1], scale=1.0 / D, r=['ss', 'epsb'], w=['rstd'])
                act(rstd[:, 0:1], rstd[:, 0:1], AF.Exp, scale=-0.5, r=['rstd'], w=['rstd'])
                stt('dve', yb, xh[s2], rstd[:, 0:1], gml, ALU.mult, ALU.mult, r=[('xh', s2), 'rstd', 'gml'], w=['yb'])

            def P_c(kt):
                s2 = kt % 2
                for kc in range(8):
                    tp(psT[:, kc * 128:(kc + 1) * 128], yb[:, kc * 128:(kc + 1) * 128], identb, r=['yb', 'identb'], w=['psT'])
                cp('act', h1T[s2], psT.rearrange("p (k t) -> p k t", k=8), r=['psT'], w=[('h1T', s2)])

            p4load_o(0)
            p4load_x(0)
            if NT > 1:
                p4load_o(1)
                p4load_x(1)
            P_a(0)
            P_a2(0)
            if NT > 1:
                P_a(1)
            P_b(0)
            P_c(0)
            for kt in range(NT):
                s2 = kt % 2
                nxt = kt + 1 < NT
                for hc in range(32):
                    pu = pU[hc % 3]
                    for kc in range(8):
                        mm(pu[:, 0:128], Wu[:, kc, hc * 128:(hc + 1) * 128], h1T[s2][:, kc, :], start=(kc == 0), stop=(kc == 7),
                           r=['Wu', ('h1T', s2)], w=[('pU', hc % 3)])
                    act(rl[hc % 2], pu[:, 0:128], AF.Relu, r=[('pU', hc % 3)], w=[('rl', hc % 2)])
                    tt('pool', hid[:, hc, :], rl[hc % 2], rl[hc % 2], ALU.mult, r=[('rl', hc % 2)], w=['hid'])
                    if nxt and hc == 3:
                        P_a2(kt + 1)
                    if nxt and hc == 6:
                        P_b(kt + 1)
                    if kt + 2 < NT and hc == 16:
                        P_a(kt + 2)
                    if nxt and hc == 26:
                        P_c(kt + 1)
                for hf in range(2):
                    for hc in range(32):
                        mm(pA[hf], hid[:, hc, :], Wd[:, hc, hf * 512:(hf + 1) * 512], start=(hc == 0), stop=(hc == 31), r=['hid', 'Wd'], w=[('pA', hf)])
                    tt('dve', xh[s2][:, hf * 512:(hf + 1) * 512], pA[hf], xh[s2][:, hf * 512:(hf + 1) * 512], ALU.add,
                       r=[('pA', hf), ('xh', s2)], w=[('xh', s2)])
                tt('dve', sq, xh[s2], xh[s2], ALU.mult, r=[('xh', s2)], w=['sq'])
                red('dve', ssm[:, 0:1], sq, ALU.add, r=['sq'], w=['ssm'])
                act(rstdm[:, 0:1], ssm[:, 0:1], AF.Ln, bias=epsb[:, 0:1], scale=1.0 / D, r=['ssm', 'epsb'], w=['rstdm'])
                act(rstdm[:, 0:1], rstdm[:, 0:1], AF.Exp, scale=-0.5, r=['rstdm'], w=['rstdm'])
                stt('dve', fin[s2], xh[s2], rstdm[:, 0:1], gfi, ALU.mult, ALU.mult, r=[('xh', s2), 'rstdm', 'gfi'], w=[('fin', s2)])
                dma(out_d[kt * 128:(kt + 1) * 128, :], fin[s2], r=[('fin', s2)], w=[('out', kt)])
                if kt + 2 < NT:
                    p4load_x(kt + 2)
    esW.close()
    S.finish()
    LIM['_cnt'] = dict(S.cnt)
    LIM['_dma'] = {q: list(v) for q, v in S.dma_use.items()}
    S.emit()
    es_all.close()
    return nc


_CONSTS = None


def make_in_maps(inputs):
    global _CONSTS
    if _CONSTS is None:
        _CONSTS = _consts()
    c = _CONSTS
    f = lambda a: np.ascontiguousarray(np.asarray(a, dtype=np.float32))
    shared = {
        'w_in': f(inputs['w_in'][0]), 'w_out': f(inputs['w_out'][0]), 'w_up': f(inputs['w_up'][0]), 'w_down': f(inputs['w_down'][0]),
        'g_attn': f(inputs['g_attn']).reshape(1, D), 'g_mlp': f(inputs['g_mlp']).reshape(1, D), 'g_final': f(inputs['g_final']).reshape(1, D),
        'g_fox': f(inputs['g_fox']).reshape(1, 512), 'g_nsa': f(inputs['g_nsa']).reshape(1, 512),
        'b_f': f(inputs['b_f']).reshape(1, 8), 'b_gate': f(inputs['b_gate']).reshape(1, 24),
        'cmpk_peT': f(np.asarray(inputs['cmpk_pe'][0]).T), 'cmpk_w1': f(inputs['cmpk_w1'][0]),
        'cmpk_b1': f(np.asarray(inputs['cmpk_b1'][0]).reshape(2, 128).T), 'cmpk_w2': f(inputs['cmpk_w2'][0]),
        'cmpk_b2': f(inputs['cmpk_b2'][0]).reshape(64, 1),
        'cmpv_peT': f(np.asarray(inputs['cmpv_pe'][0]).T), 'cmpv_w1': f(inputs['cmpv_w1'][0]),
        'cmpv_b1': f(np.asarray(inputs['cmpv_b1'][0]).reshape(2, 128).T), 'cmpv_w2': f(inputs['cmpv_w2'][0]),
        'cmpv_b2': f(inputs['cmpv_b2'][0]).reshape(1, 64),
    }
    shared.update(c)
    x = np.asarray(inputs['x'], dtype=np.float32)
    return [dict(shared, x=np.ascontiguousarray(x[b])) for b in range(x.shape[0])]


def kernel(**inputs):
    nc = build()
    in_maps = make_in_maps(inputs)
    res = run_bass_kernel_spmd(nc, in_maps, core_ids=list(range(8)))
    return np.stack([np.asarray(r['out'], dtype=np.float32) for r in res.results], axis=0)
```
